# Optimizing a Trainium2 kernel written in Bass

```python
import math
import jax, jax.numpy as jnp
from jax import lax
import numpy as np


D_MODEL = 1024
BATCH = 8
SEQ = 4096
DEPTH = 1

MIX_WIDTH = D_MODEL
NSA_WIDTH = MIX_WIDTH // 2
NSA_HEAD_DIM = 64
NSA_HEADS = NSA_WIDTH // NSA_HEAD_DIM
NSA_KV_HEADS = 2
NSA_GROUP = NSA_HEADS // NSA_KV_HEADS
NSA_KV_WIDTH = NSA_KV_HEADS * NSA_HEAD_DIM
CMP_STRIDE = 16
CMP_BLOCK = 2 * CMP_STRIDE
CMP_HIDDEN = 4 * NSA_HEAD_DIM
SEL_BLOCK = 64
N_SELECT = 16
WINDOW = 512
Q_BLOCK = 128
FORCED_SCORE = 1.0e4
MLSTM_WIDTH = MIX_WIDTH - NSA_WIDTH
MLSTM_HEADS = 4
MLSTM_V_DIM = MLSTM_WIDTH // MLSTM_HEADS
MLSTM_QK_DIM = MLSTM_V_DIM // 2
MLSTM_QK_WIDTH = MLSTM_HEADS * MLSTM_QK_DIM
MLSTM_CHUNK = 64
CONV_WIDTH = 4
REL_BUCKETS = 32
REL_MAX_DISTANCE = 128
MEM_TOKENS = 256
XATTN_HEADS = 4
XATTN_HEAD_DIM = D_MODEL // XATTN_HEADS
D_FF = -(-8 * D_MODEL // (3 * 256)) * 256
NORM_EPS = 1e-6
NEG_INF = -1.0e30

IN_SIZES = (NSA_WIDTH,) + (NSA_KV_WIDTH,) * 6 + (NSA_HEADS * 3, MLSTM_QK_WIDTH, MLSTM_QK_WIDTH,
            MLSTM_WIDTH, MLSTM_HEADS, MLSTM_HEADS, MLSTM_WIDTH)
IN_WIDTH = sum(IN_SIZES)
IN_OFFSETS = tuple(int(o) for o in np.cumsum(IN_SIZES)[:-1])

kernel_name = 'hymba_nsa_mlstm_hybrid_layer'


def rms_norm(x, gain):
    xf = x.astype(jnp.float32)
    y = xf * lax.rsqrt(jnp.mean(xf * xf, axis=-1, keepdims=True) + NORM_EPS)
    return (y * gain.astype(jnp.float32)).astype(x.dtype)


def rel_bucket(dist):
    n = jnp.maximum(dist, 0)
    max_exact = REL_BUCKETS // 2
    nf = jnp.maximum(n, 1).astype(jnp.float32)
    large = max_exact + (jnp.log(nf / max_exact) / math.log(REL_MAX_DISTANCE / max_exact)
                         * (REL_BUCKETS - max_exact)).astype(jnp.int32)
    large = jnp.minimum(large, REL_BUCKETS - 1)
    return jnp.where(n < max_exact, n, large)


def masked_softmax(s, mask):
    s = jnp.where(mask, s.astype(jnp.float32), NEG_INF)
    return jnp.where(mask, jax.nn.softmax(s, axis=-1), 0.0)


def compress_blocks(kv, pos, w1, w2):
    B, H, S, DH = kv.shape
    c = kv.reshape(B, H, S // CMP_STRIDE, CMP_STRIDE, DH)
    blocks = jnp.concatenate([c[:, :, :-1], c[:, :, 1:]], axis=3) + pos
    hid = jax.nn.silu(blocks.reshape(B, H, -1, CMP_BLOCK * DH) @ w1)
    return hid @ w2


def native_sparse_attention(q, k_cmp, v_cmp, k_slc, v_slc, k_win, v_win, gates, rel_bias):
    B, HKV, G, S, DH = q.shape
    n_cmp = k_cmp.shape[2]
    n_sel = S // SEL_BLOCK
    n_top = min(N_SELECT, n_sel)
    n_kwin = Q_BLOCK + WINDOW
    cmp_start = jnp.arange(n_cmp) * CMP_STRIDE
    cmp_end = cmp_start + (CMP_BLOCK - 1)
    sel_start = jnp.arange(n_sel) * SEL_BLOCK
    overlap = ((cmp_start[:, None] <= sel_start[None, :] + SEL_BLOCK - 1)
               & (cmp_end[:, None] >= sel_start[None, :])).astype(jnp.float32)
    k_blk = k_slc.reshape(B, HKV, n_sel, SEL_BLOCK, DH)
    v_blk = v_slc.reshape(B, HKV, n_sel, SEL_BLOCK, DH)
    k_pad = jnp.pad(k_win, ((0, 0), (0, 0), (WINDOW, 0), (0, 0)))
    v_pad = jnp.pad(v_win, ((0, 0), (0, 0), (WINDOW, 0), (0, 0)))
    gather = jax.vmap(jax.vmap(lambda blocks, idx: blocks[idx]))
    head_off = (jnp.arange(NSA_HEADS) * REL_BUCKETS).reshape(HKV, G)
    bias_flat = rel_bias.reshape(-1)
    blk_ids = jnp.arange(n_sel)

    def head_bias(dist):
        return rel_bias[:, rel_bucket(dist)].reshape(HKV, G, *dist.shape)

    def query_block(c):
        t0 = c * Q_BLOCK
        tq = t0 + jnp.arange(Q_BLOCK)
        qc = lax.dynamic_slice_in_dim(q, t0, Q_BLOCK, axis=3)
        gc = lax.dynamic_slice_in_dim(gates, t0, Q_BLOCK, axis=3)
        d_c = tq[:, None] - cmp_end[None, :]
        s_c = jnp.einsum('bhgtd,bhcd->bhgtc', qc, k_cmp).astype(jnp.float32) + head_bias(d_c)
        p_c = masked_softmax(s_c, d_c >= 0)
        o_c = jnp.einsum('bhgtc,bhcd->bhgtd', p_c.astype(v_cmp.dtype), v_cmp)
        imp = jnp.einsum('bhgtc,cj->bhtj', p_c, overlap)
        cur = tq // SEL_BLOCK
        forced = ((blk_ids[None, :] == 0) | (blk_ids[None, :] == cur[:, None])
                  | (blk_ids[None, :] == cur[:, None] - 1))
        score = jnp.where(forced, FORCED_SCORE, jnp.where(blk_ids[None, :] <= cur[:, None], imp, -1.0))
        _, idx = lax.top_k(score, n_top)
        k_s = gather(k_blk, idx).reshape(B, HKV, Q_BLOCK, n_top * SEL_BLOCK, DH)
        v_s = gather(v_blk, idx).reshape(B, HKV, Q_BLOCK, n_top * SEL_BLOCK, DH)
        kpos = (idx[..., None] * SEL_BLOCK + jnp.arange(SEL_BLOCK)).reshape(B, HKV, Q_BLOCK, n_top * SEL_BLOCK)
        d_s = tq[:, None] - kpos
        b_s = bias_flat[head_off[None, :, :, None, None] + rel_bucket(d_s)[:, :, None]]
        s_s = jnp.einsum('bhgtd,bhtkd->bhgtk', qc, k_s).astype(jnp.float32) + b_s
        p_s = masked_softmax(s_s, (d_s >= 0)[:, :, None])
        o_s = jnp.einsum('bhgtk,bhtkd->bhgtd', p_s.astype(v_s.dtype), v_s)
        k_w = lax.dynamic_slice_in_dim(k_pad, t0, n_kwin, axis=2)
        v_w = lax.dynamic_slice_in_dim(v_pad, t0, n_kwin, axis=2)
        kpos_w = t0 - WINDOW + jnp.arange(n_kwin)
        d_w = tq[:, None] - kpos_w[None, :]
        m_w = (d_w >= 0) & (d_w < WINDOW) & (kpos_w[None, :] >= 0)
        s_w = jnp.einsum('bhgtd,bhkd->bhgtk', qc, k_w).astype(jnp.float32) + head_bias(d_w)
        p_w = masked_softmax(s_w, m_w)
        o_w = jnp.einsum('bhgtk,bhkd->bhgtd', p_w.astype(v_w.dtype), v_w)
        return gc[..., 0:1] * o_c + gc[..., 1:2] * o_s + gc[..., 2:3] * o_w

    return lax.map(query_block, jnp.arange(S // Q_BLOCK))


def causal_depthwise_conv(u, w, b):
    y = lax.conv_general_dilated(u, w[:, None, :].astype(u.dtype), window_strides=(1,),
                                 padding=((CONV_WIDTH - 1, 0),), dimension_numbers=('NWC', 'WIO', 'NWC'),
                                 feature_group_count=u.shape[-1])
    return y + b


def mlstm_chunkwise(q, k, v, i_pre, log_f):
    B, S, H, DK = q.shape
    DV = v.shape[-1]
    L = MLSTM_CHUNK
    nc = S // L
    f32 = jnp.float32

    def chunks(t):
        return t.astype(f32).reshape(B, nc, L, H, -1).transpose(1, 0, 3, 2, 4)

    qs, ks, vs = chunks(q), chunks(k) * (DK ** -0.5), chunks(v)
    ig = chunks(i_pre[..., None])[..., 0]
    lf = chunks(log_f[..., None])[..., 0]
    causal = jnp.tril(jnp.ones((L, L), dtype=bool))

    def step(carry, inp):
        C, n, m = carry
        qc, kc, vc, ic, fc = inp
        b = jnp.cumsum(fc, axis=-1)
        D = jnp.where(causal, b[..., :, None] - b[..., None, :] + ic[..., None, :], NEG_INF)
        m_inter = b + m[..., None]
        m_t = jnp.maximum(m_inter, jnp.max(D, axis=-1))
        w = jnp.exp(D - m_t[..., None])
        a = jnp.exp(m_inter - m_t)
        s = jnp.einsum('bhtd,bhsd->bhts', qc, kc) * w
        num = jnp.einsum('bhts,bhsv->bhtv', s, vc) + a[..., None] * jnp.einsum('bhtd,bhdv->bhtv', qc, C)
        den = jnp.sum(s, axis=-1) + a * jnp.einsum('bhtd,bhd->bht', qc, n)
        h = num / jnp.maximum(jnp.abs(den), jnp.exp(-m_t))[..., None]
        bL = b[..., -1]
        ws = bL[..., None] - b + ic
        m_next = jnp.maximum(bL + m, jnp.max(ws, axis=-1))
        decay = jnp.exp(bL + m - m_next)
        kw = kc * jnp.exp(ws - m_next[..., None])[..., None]
        C = decay[..., None, None] * C + jnp.einsum('bhsd,bhsv->bhdv', kw, vc)
        n = decay[..., None] * n + jnp.sum(kw, axis=2)
        return (C, n, m_next), h

    init = (jnp.zeros((B, H, DK, DV), f32), jnp.zeros((B, H, DK), f32), jnp.zeros((B, H), f32))
    _, hs = lax.scan(step, init, (qs, ks, vs, ig, lf))
    return hs.transpose(1, 0, 3, 2, 4).reshape(B, S, H, DV)


def hybrid_mixer(h, rel_bias, w_in, cmp_pos_k, cmp_pos_v, cmp_w1_k, cmp_w2_k, cmp_w1_v, cmp_w2_v,
                 conv_w, conv_b, mlstm_gate_bias, mlstm_norm, w_out):
    B, S, _ = h.shape
    (nq, kc, vc, ksl, vsl, kwn, vwn, gt, mq, mk, mv, mi, mf, mo) = jnp.split(h @ w_in, IN_OFFSETS, axis=-1)

    def kv_heads(t):
        return t.reshape(B, S, NSA_KV_HEADS, NSA_HEAD_DIM).transpose(0, 2, 1, 3)

    q = nq.reshape(B, S, NSA_KV_HEADS, NSA_GROUP, NSA_HEAD_DIM).transpose(0, 2, 3, 1, 4) * (NSA_HEAD_DIM ** -0.5)
    k_cmp = compress_blocks(kv_heads(kc), cmp_pos_k, cmp_w1_k, cmp_w2_k)
    v_cmp = compress_blocks(kv_heads(vc), cmp_pos_v, cmp_w1_v, cmp_w2_v)
    gates = jax.nn.sigmoid(gt.reshape(B, S, NSA_KV_HEADS, NSA_GROUP, 3).transpose(0, 2, 3, 1, 4))
    o = native_sparse_attention(q, k_cmp, v_cmp, kv_heads(ksl), kv_heads(vsl), kv_heads(kwn), kv_heads(vwn),
                                gates, rel_bias)
    y_nsa = o.transpose(1, 0, 4, 2, 3, 5).reshape(B, S, NSA_WIDTH)

    qk = jax.nn.silu(causal_depthwise_conv(jnp.concatenate([mq, mk], axis=-1), conv_w, conv_b))
    mq, mk = jnp.split(qk, 2, axis=-1)
    hm = mlstm_chunkwise(mq.reshape(B, S, MLSTM_HEADS, MLSTM_QK_DIM), mk.reshape(B, S, MLSTM_HEADS, MLSTM_QK_DIM),
                         mv.reshape(B, S, MLSTM_HEADS, MLSTM_V_DIM),
                         mi.astype(jnp.float32) + mlstm_gate_bias[0].astype(jnp.float32),
                         jax.nn.log_sigmoid(mf.astype(jnp.float32) + mlstm_gate_bias[1].astype(jnp.float32)))
    hm = hm * lax.rsqrt(jnp.mean(hm * hm, axis=-1, keepdims=True) + NORM_EPS)
    hm = (hm.reshape(B, S, MLSTM_WIDTH) * mlstm_norm.astype(jnp.float32)).astype(h.dtype)
    y_ml = jax.nn.sigmoid(mo) * hm

    return jnp.concatenate([y_nsa, y_ml], axis=-1) @ w_out


def memory_cross_attention(h, mem_n, w_xq, w_xkv, w_xo):
    B, S, _ = h.shape
    M = mem_n.shape[1]
    q = (h @ w_xq).reshape(B, S, XATTN_HEADS, XATTN_HEAD_DIM) * (XATTN_HEAD_DIM ** -0.5)
    k, v = jnp.split(mem_n @ w_xkv, 2, axis=-1)
    k = k.reshape(B, M, XATTN_HEADS, XATTN_HEAD_DIM)
    v = v.reshape(B, M, XATTN_HEADS, XATTN_HEAD_DIM)
    p = jax.nn.softmax(jnp.einsum('bshd,bmhd->bhsm', q, k).astype(jnp.float32), axis=-1)
    o = jnp.einsum('bhsm,bmhd->bshd', p.astype(v.dtype), v).reshape(B, S, D_MODEL)
    return o @ w_xo


def swiglu_ffn(h, w_gate_up, w_down):
    g, u = jnp.split(h @ w_gate_up, 2, axis=-1)
    return (jax.nn.silu(g) * u) @ w_down


def setup_inputs(seed: int = 0) -> dict:
    key = jax.random.key(seed)
    keys = iter(jax.random.split(key, 40))

    def nrm(shape, scale):
        return scale * jax.random.normal(next(keys), shape, jnp.float32)

    def gain(width=D_MODEL):
        return 1.0 + nrm((DEPTH, width), 0.02)

    inputs = {}
    inputs['x'] = nrm((BATCH, SEQ, D_MODEL), 1.0)
    inputs['mem'] = nrm((BATCH, MEM_TOKENS, D_MODEL), 1.0)
    inputs['rel_bias'] = nrm((NSA_HEADS, REL_BUCKETS), 0.5)
    inputs['mix_norm_pre'] = gain()
    inputs['w_in'] = nrm((DEPTH, D_MODEL, IN_WIDTH), D_MODEL ** -0.5)
    inputs['cmp_pos_k'] = nrm((DEPTH, CMP_BLOCK, NSA_HEAD_DIM), 0.02)
    inputs['cmp_pos_v'] = nrm((DEPTH, CMP_BLOCK, NSA_HEAD_DIM), 0.02)
    inputs['cmp_w1_k'] = nrm((DEPTH, CMP_BLOCK * NSA_HEAD_DIM, CMP_HIDDEN), (CMP_BLOCK * NSA_HEAD_DIM) ** -0.5)
    inputs['cmp_w2_k'] = nrm((DEPTH, CMP_HIDDEN, NSA_HEAD_DIM), CMP_HIDDEN ** -0.5)
    inputs['cmp_w1_v'] = nrm((DEPTH, CMP_BLOCK * NSA_HEAD_DIM, CMP_HIDDEN), (CMP_BLOCK * NSA_HEAD_DIM) ** -0.5)
    inputs['cmp_w2_v'] = nrm((DEPTH, CMP_HIDDEN, NSA_HEAD_DIM), CMP_HIDDEN ** -0.5)
    inputs['conv_w'] = nrm((DEPTH, CONV_WIDTH, 2 * MLSTM_QK_WIDTH), CONV_WIDTH ** -0.5)
    inputs['conv_b'] = nrm((DEPTH, 2 * MLSTM_QK_WIDTH), 0.01)
    forget_bias = jnp.broadcast_to(jnp.linspace(3.0, 6.0, MLSTM_HEADS), (DEPTH, MLSTM_HEADS))
    inputs['mlstm_gate_bias'] = jnp.stack([nrm((DEPTH, MLSTM_HEADS), 0.1),
                                           forget_bias + nrm((DEPTH, MLSTM_HEADS), 0.1)], axis=1)
    inputs['mlstm_norm'] = gain(MLSTM_WIDTH)
    inputs['w_out'] = nrm((DEPTH, MIX_WIDTH, D_MODEL), MIX_WIDTH ** -0.5)
    inputs['mix_norm_post'] = gain()
    inputs['xattn_norm_pre'] = gain()
    inputs['mem_norm'] = gain()
    inputs['w_xq'] = nrm((DEPTH, D_MODEL, D_MODEL), D_MODEL ** -0.5)
    inputs['w_xkv'] = nrm((DEPTH, D_MODEL, 2 * D_MODEL), D_MODEL ** -0.5)
    inputs['w_xo'] = nrm((DEPTH, D_MODEL, D_MODEL), D_MODEL ** -0.5)
    inputs['xattn_norm_post'] = gain()
    inputs['ffn_norm_pre'] = gain()
    inputs['w_gate_up'] = nrm((DEPTH, D_MODEL, 2 * D_FF), D_MODEL ** -0.5)
    inputs['w_down'] = nrm((DEPTH, D_FF, D_MODEL), D_FF ** -0.5)
    inputs['ffn_norm_post'] = gain()
    return inputs


def reference(x, mem, rel_bias, mix_norm_pre, w_in, cmp_pos_k, cmp_pos_v, cmp_w1_k, cmp_w2_k, cmp_w1_v,
              cmp_w2_v, conv_w, conv_b, mlstm_gate_bias, mlstm_norm, w_out, mix_norm_post, xattn_norm_pre,
              mem_norm, w_xq, w_xkv, w_xo, xattn_norm_post, ffn_norm_pre, w_gate_up, w_down, ffn_norm_post):
    for l in range(DEPTH):
        h = rms_norm(x, mix_norm_pre[l])
        y = hybrid_mixer(h, rel_bias, w_in[l], cmp_pos_k[l], cmp_pos_v[l], cmp_w1_k[l], cmp_w2_k[l],
                         cmp_w1_v[l], cmp_w2_v[l], conv_w[l], conv_b[l], mlstm_gate_bias[l], mlstm_norm[l], w_out[l])
        x = x + rms_norm(y, mix_norm_post[l])
        h = rms_norm(x, xattn_norm_pre[l])
        y = memory_cross_attention(h, rms_norm(mem, mem_norm[l]), w_xq[l], w_xkv[l], w_xo[l])
        x = x + rms_norm(y, xattn_norm_post[l])
        h = rms_norm(x, ffn_norm_pre[l])
        y = swiglu_ffn(h, w_gate_up[l], w_down[l])
        x = x + rms_norm(y, ffn_norm_post[l])
    return x
```

```python
import contextlib
import numpy as np
import ml_dtypes
import concourse.bass as bass
import concourse.mybir as mybir
from concourse.bass_utils import run_bass_kernel_spmd

F32 = mybir.dt.float32
BF16 = mybir.dt.bfloat16
AF = mybir.ActivationFunctionType
ALU = mybir.AluOpType
AX = mybir.AxisListType
NPBF = ml_dtypes.bfloat16

D = 1024
NEG = -30000.0
ENGS = ("pe", "act", "dve", "pool", "sp")
REORDER = True
FILLERS = True


class _Rec:
    def __init__(self):
        self.call = None

    def __getattr__(self, name):
        def f(*a, **kw):
            self.call = (name, a, kw)
            return None
        return f


class Sched:
    def __init__(self, nc):
        self.nc = nc
        self.ops = []
        self.last_writer = {}
        self.readers = {}
        self.bounds = []
        self.filler = None
        self.nfill = 0

    def barrier(self):
        self.bounds.append(len(self.ops))

    @staticmethod
    def _cost(eng, meth, a, kw):
        def fsz(ap):
            n = 1
            for d in ap.shape[1:]:
                n *= int(d)
            return n
        try:
            if meth == "matmul":
                n = fsz(kw["rhs"])
                t = max(0.07, n * 0.0006)
                if kw["rhs"].dtype == F32:
                    t *= 4
                return t, t + 0.1
            if meth == "transpose" and eng == "pe":
                return 0.12, 0.3
            if meth == "dma_start":
                ap = kw["out"]
                n = fsz(ap) * int(ap.shape[0])
                return 0.15, 3.0 + n * 4 / 150e3
            ap = kw.get("out", kw.get("ap", a[0] if a else None))
            n = fsz(ap)
            if eng == "act":
                t = 0.22 + n * 0.00078
            elif eng == "dve":
                t = 0.15 + n * 0.00105
            else:
                t = 0.3 + n * 0.0034
            return t, t + 0.15
        except Exception:
            return 0.3, 0.5

    def _schedule_segment(self, lo, hi):
        import heapq
        ops = self.ops
        ndeps = {}
        users = {}
        for i in range(lo, hi):
            ds = [d for d in ops[i]["deps"] if d >= lo]
            ndeps[i] = len(ds)
            for d in ds:
                users.setdefault(d, []).append(i)
        tfree = {e: 0.0 for e in ENGS}
        Ht = {e: [] for e in ENGS}
        Hi = {e: [] for e in ENGS}
        end = {}
        ready = {}
        for i in range(lo, hi):
            if ndeps[i] == 0:
                ready[i] = 0.0
                heapq.heappush(Ht[ops[i]["eng"]], (0.0, i))
        order = []
        nleft = hi - lo
        pemode = [None]
        SWITCH = 0.25
        while nleft:
            best = None
            for e in ENGS:
                while Ht[e] and Ht[e][0][0] <= tfree[e]:
                    _, i = heapq.heappop(Ht[e])
                    heapq.heappush(Hi[e], i)
                if e == "pe" and Hi[e] and ops[Hi[e][0]]["mode"] != pemode[0]:
                    same = [j for j in Hi[e][:24] if ops[j]["mode"] == pemode[0]]
                    if same:
                        j = min(same)
                        Hi[e].remove(j)
                        heapq.heapify(Hi[e])
                        cand = (tfree[e], j, e, None)
                    else:
                        cand = (tfree[e] + SWITCH, Hi[e][0], e, True)
                elif Hi[e]:
                    cand = (tfree[e], Hi[e][0], e, True)
                elif Ht[e]:
                    cand = (Ht[e][0][0], Ht[e][0][1], e, False)
                else:
                    continue
                if cand[3] is None:
                    pass
                if best is None or cand[:2] < best[:2]:
                    if best is not None and best[3] is None:
                        heapq.heappush(Hi[best[2]], best[1])
                    best = cand
                elif cand[3] is None:
                    heapq.heappush(Hi[e], cand[1])
            if self.filler is not None and self.filler[0] == self._cur_seg:
                pec = None
                for e2 in ("pe",):
                    if Hi[e2]:
                        pec = tfree[e2]
                    elif Ht[e2]:
                        pec = Ht[e2][0][0]
                fdep = self.filler[2]
                if pec is not None and fdep in end and pec - max(tfree["pe"], end[fdep]) >= 0.45 and best[0] >= tfree["pe"]:
                    fi = len(ops)
                    ops.append(dict(eng="pe", fn=self.filler[1], deps=[fdep], dma=None, fence=False, cost=(0.22, 0.3), mode="full",
                                    filler=True))
                    stf = max(tfree["pe"], end[fdep])
                    tfree["pe"] = stf + 0.22
                    end[fi] = stf + 0.3
                    order.append(fi)
                    pemode[0] = "full"
                    self.nfill += 1
                    if best[3] is None:
                        heapq.heappush(Hi[best[2]], best[1])
                    continue
            st, i, e, fromidx = best
            if fromidx is None:
                pass
            elif fromidx:
                heapq.heappop(Hi[e])
            else:
                heapq.heappop(Ht[e])
            if e == "pe":
                pemode[0] = ops[i]["mode"]
            busy, lat = ops[i]["cost"]
            tfree[e] = st + busy
            end[i] = st + lat
            order.append(i)
            nleft -= 1
            for u in users.get(i, ()):
                ndeps[u] -= 1
                ready[u] = max(ready.get(u, 0.0), end[i])
                if ndeps[u] == 0:
                    heapq.heappush(Ht[ops[u]["eng"]], (ready[u], u))
        self.makespan = getattr(self, "makespan", 0.0) + max(end.values())
        return order

    def finalize(self, reorder=True):
        ops = self.ops
        bounds = [0] + self.bounds + [len(ops)]
        order = []
        nreal = len(ops)
        bounds[-1] = nreal
        for si_, (lo, hi) in enumerate(zip(bounds[:-1], bounds[1:])):
            self._cur_seg = si_
            if hi > lo:
                seg_order = self._schedule_segment(lo, hi) if reorder else list(range(lo, hi))
                for i_ in seg_order:
                    ops[i_]["seg_override"] = si_
                order += seg_order
        newidx = {old: new for new, old in enumerate(order)}
        seg_of = {}
        for si, (lo, hi) in enumerate(zip(bounds[:-1], bounds[1:])):
            for i in range(lo, hi):
                seg_of[i] = si
        newops = []
        for old in order:
            o = ops[old]
            o["deps"] = sorted(newidx[d] for d in o["deps"])
            o["seg"] = o.get("seg_override", seg_of.get(old, 0))
            newops.append(o)
        last = {}
        seen = set()
        cur = 0
        for i, o in enumerate(newops):
            if o["seg"] != cur:
                cur = o["seg"]
                seen = set()
                self._bar = set(last.values())
            if cur > 0 and o["eng"] not in seen:
                seen.add(o["eng"])
                o["deps"] = sorted(set(o["deps"]) | self._bar)
            if o["dma"] is not None:
                last[("dma", o["dma"])] = i
            else:
                last[("eng", o["eng"])] = i
        self.ops = newops

    def op(self, eng, fn, reads=(), writes=(), dma=None, fence=False):
        i = len(self.ops)
        writes = list(writes) + [k for k in reads if isinstance(k, str) and k.startswith("pb")]
        reads = [k for k in reads if not (isinstance(k, str) and k.startswith("pb"))]
        deps = set()
        for k in list(reads) + list(writes):
            w = self.last_writer.get(k)
            if w is not None:
                deps.add(w)
        for k in writes:
            for r in self.readers.get(k, ()):
                deps.add(r)
        deps.discard(i)
        for k in writes:
            self.last_writer[k] = i
            self.readers[k] = []
        for k in reads:
            self.readers.setdefault(k, []).append(i)
        rec = _Rec()
        fn(rec)
        assert rec.call is not None
        meth, a, kw = rec.call
        mode = None
        if eng == "pe":
            try:
                l_ = kw.get("lhsT", kw.get("in_"))
                mode = "full" if (int(l_.shape[0]) == 128 and l_.dtype != F32) else "tile"
            except Exception:
                mode = "tile"
        self.ops.append(dict(eng=eng, fn=(lambda e: getattr(e, meth)(*a, **kw)), deps=sorted(deps), dma=dma, fence=fence,
                             cost=self._cost(eng, meth, a, kw), mode=mode))
        return i

    def emit(self, reorder=True):
        self.finalize(reorder)
        nc = self.nc
        ops = self.ops
        needed = [False] * len(ops)
        for o in ops:
            for d in o["deps"]:
                od = ops[d]
                if od["dma"] is None and od["eng"] == "pe" and o["eng"] == "pe" and o["dma"] is None and not o["fence"]:
                    continue
                needed[d] = True
        eng_cnt = {e: 0 for e in ENGS}
        dma_cnt = {}
        for i, o in enumerate(ops):
            if o["dma"] is not None:
                k = o["dma"]
                dma_cnt[k] = dma_cnt.get(k, 0) + 16
                o["sem"] = ("dma", k)
                o["val"] = dma_cnt[k]
            else:
                e = o["eng"]
                if needed[i]:
                    eng_cnt[e] += 1
                    o["sem"] = ("eng", e)
                    o["val"] = eng_cnt[e]
                else:
                    o["sem"] = None
        sem_names = [("eng", e) for e in ENGS if e != "sp"] + [("dma", k) for k in dma_cnt]
        with contextlib.ExitStack() as st:
            sems = {}
            for sn in sem_names:
                sems[sn] = st.enter_context(nc.semaphore("s_%s_%s" % sn))
            block = st.enter_context(nc.Block())
            per_eng = {e: [] for e in ENGS}
            for i, o in enumerate(ops):
                per_eng[o["eng"]].append(i)
            final_dma = dict(dma_cnt)

            def make_body(e):
                def body(eng):
                    waited = {}
                    for i in per_eng[e]:
                        o = ops[i]
                        need = {}
                        for d in o["deps"]:
                            od = ops[d]
                            if od["sem"] is None:
                                continue
                            if od["sem"] == ("eng", "pe") and e == "pe" and o["dma"] is None and not o["fence"]:
                                continue
                            sn, v = od["sem"], od["val"]
                            if v > need.get(sn, 0):
                                need[sn] = v
                        for sn, v in need.items():
                            if waited.get(sn, 0) >= v:
                                continue
                            waited[sn] = v
                            eng.wait_ge(sems[sn], v)
                        ins = o["fn"](eng)
                        if o["sem"] is not None:
                            ins.then_inc(sems[o["sem"]], 16 if o["dma"] is not None else 1)
                    if e == "sp":
                        for k, v in final_dma.items():
                            eng.wait_ge(sems[("dma", k)], v)
                return body

            block.tensor(make_body("pe"))
            block.scalar(make_body("act"))
            block.vector(make_body("dve"))
            block.gpsimd(make_body("pool"))
            block.sync(make_body("sp"))


def rel_bucket_np(dist):
    n = np.maximum(dist, 0)
    nf = np.maximum(n, 1).astype(np.float32)
    large = 16 + (np.log(nf / np.float32(16)) / np.float32(np.log(128 / 16)) * np.float32(16)).astype(np.int32)
    large = np.minimum(large, 31)
    return np.where(n < 16, n, large)


def host_consts():
    c = {}
    c["ident_f"] = np.eye(128, dtype=np.float32)
    c["ident_b"] = np.eye(128, dtype=np.float32).astype(NPBF)
    c["anti_b"] = np.eye(128, dtype=np.float32)[::-1].copy().astype(NPBF)
    c["ones_b"] = np.ones((128, 128), np.float32).astype(NPBF)
    idx = np.arange(384)
    dist = idx - 127
    oh = np.zeros((33, 384), np.float32)
    b = rel_bucket_np(dist)
    for i in range(384):
        if dist[i] >= 0:
            oh[b[i], i] = 1.0
            oh[31, i] -= 1.0
        else:
            oh[32, i] = 1.0
    c["onehot"] = oh
    kl = np.arange(128)[:, None]
    tl = np.arange(128)[None, :]
    c["d4"] = np.where(kl > tl, 0.0, NEG).astype(np.float32).astype(NPBF)
    sw = np.zeros((17, 384), np.float32)
    for k in range(16):
        sw[k, 254 - k] = 1.0
    sw[16, 255:] = 1.0
    c["shiftw"] = sw.astype(NPBF)
    tlv = np.arange(128)
    c0 = (tlv >= 64).astype(np.int64)
    k = np.arange(126)[None, :] - 62
    forced = (k == c0[:, None]) | (k == c0[:, None] - 1)
    valid = k <= c0[:, None]
    vm = (valid & ~forced).astype(np.float32)
    add = np.where(forced, 1.0e4, np.where(valid, 0.0, -1.0)).astype(np.float32)
    c["vm_tab"] = vm
    c["add_tab"] = add
    va = np.zeros((128, 2, 2, 129), np.float32)
    for ch in range(2):
        for p in range(128):
            cc = ch * 128 + p
            if cc >= 255:
                continue
            va[p, :, ch, 64] = 1.0
            for j in range(64):
                if 16 * cc <= 64 * j + 63 and 16 * cc + 31 >= 64 * j:
                    va[p, :, ch, 65 + j] = 1.0
    c["vcmp_init"] = va.astype(NPBF)
    s = np.arange(128)[:, None] % 64
    t = np.arange(64)[None, :]
    c["tri_ml"] = (s <= t).astype(np.float32)
    rm = np.ones((4, 512), np.float32)
    rm[:, 0::64] = 0.0
    c["resetmask"] = rm
    nr = np.zeros((4, 512), np.float32)
    nr[:, 0::64] = -1.0e30
    c["negreset"] = nr
    selbc = np.zeros((4, 4, 64), np.float32)
    for h in range(4):
        selbc[h, h, :] = 1.0
    c["selbc"] = selbc.astype(NPBF)
    selpair = np.zeros((4, 2, 128), np.float32)
    for cp in range(2):
        selpair[2 * cp, cp, 0:64] = 1.0
        selpair[2 * cp + 1, cp, 64:128] = 1.0
    c["selpair"] = selpair
    ex = np.zeros((64, 4096), np.float32)
    for j in range(64):
        ex[j, 64 * j:64 * j + 64] = 1.0
    c["expand"] = ex.astype(NPBF)
    return c


CONST_NAMES = ["ident_f", "ident_b", "anti_b", "ones_b", "onehot", "d4", "shiftw", "vm_tab", "add_tab",
               "vcmp_init", "tri_ml", "resetmask", "negreset", "selbc", "selpair", "expand"]


def np_dt(a):
    return BF16 if a.dtype == NPBF else F32


def build_program(S_TOK=4096, phases=("A", "B1", "B2"), debug=False):
    NT = S_TOK // 128
    NST = S_TOK // 512
    nc = bass.Bass("TRN2", target_bir_lowering=False)
    consts_np = host_consts()

    def din(name, shape, dt=F32):
        return nc.dram_tensor(name, list(shape), dt, kind="ExternalInput").ap()

    x_d = din("x", [S_TOK, D])
    mem_d = din("mem", [256, D])
    relb_d = din("rel_biasT", [33, 8])
    w_in_d = din("w_in_p", [D, 3104])
    gpre_d = din("mix_norm_pre", [1, D])
    posk_d = din("cmp_pos_k", [128, 16])
    posv_d = din("cmp_pos_v", [128, 16])
    w1k_d = din("cmp_w1_k", [2048, 256])
    w1v_d = din("cmp_w1_v", [2048, 256])
    w2k_d = din("cmp_w2_k", [256, 64])
    w2v_d = din("cmp_w2_v", [256, 64])
    convw_d = din("conv_w_p", [128, 4, 4])
    convb_d = din("conv_b_p", [128, 4])
    gb_d = din("gate_bias_p", [4, 2])
    mln_d = din("mlstm_norm", [1, 512])
    w_out_d = din("w_out", [D, D])
    gpost_d = din("mix_norm_post", [1, D])
    gxpre_d = din("xattn_norm_pre", [1, D])
    gmem_d = din("mem_norm", [1, D])
    w_xq_d = din("w_xq", [D, D])
    w_xkv_d = din("w_xkv", [D, 2 * D])
    w_xo_d = din("w_xo", [D, D])
    gxpost_d = din("xattn_norm_post", [1, D])
    gfpre_d = din("ffn_norm_pre", [1, D])
    w_gu_d = din("w_gate_up", [D, 5632])
    w_dn_d = din("w_down", [2816, D])
    gfpost_d = din("ffn_norm_post", [1, D])
    cd = {n: din("c_" + n, consts_np[n].shape, np_dt(consts_np[n])) for n in CONST_NAMES}

    out_d = nc.dram_tensor("out", [S_TOK, D], F32, kind="ExternalOutput").ap()
    ycat_d = nc.dram_tensor("ycat_scr", [S_TOK, D], BF16, kind="Internal").ap()
    x2_d = nc.dram_tensor("x2_scr", [S_TOK, D], F32, kind="Internal").ap()
    F_d = nc.dram_tensor("F_scr", [8, 384], F32, kind="Internal").ap()
    dbg = {}
    if debug:
        dbg["ycat"] = nc.dram_tensor("dbg_ycat", [S_TOK, D], BF16, kind="ExternalOutput").ap()
        dbg["x2"] = nc.dram_tensor("dbg_x2", [S_TOK, D], F32, kind="ExternalOutput").ap()

    S = Sched(nc)
    rr = [0]

    def alt():
        rr[0] ^= 1
        return "act" if rr[0] else "dve"

    def evac(eng, out, in_, reads, writes, scale=None):
        if eng == "act":
            if scale is None:
                S.op("act", lambda e: e.copy(out=out, in_=in_), reads=reads, writes=writes)
            else:
                S.op("act", lambda e: e.activation(out=out, in_=in_, func=AF.Copy, scale=scale), reads=reads, writes=writes)
        else:
            if scale is None:
                S.op("dve", lambda e: e.tensor_copy(out=out, in_=in_), reads=reads, writes=writes)
            else:
                S.op("dve", lambda e: e.tensor_scalar_mul(out=out, in0=in_, scalar1=scale), reads=reads, writes=writes)

    def mm(out, lhsT, rhs, start, stop, reads, writes, skip=False, fence=False):
        S.op("pe", lambda e: e.matmul(out, lhsT=lhsT, rhs=rhs, start=start, stop=stop, skip_group_check=skip),
             reads=reads, writes=writes, fence=fence)

    with contextlib.ExitStack() as top:
        pb = [top.enter_context(nc.psum_tensor("pb%d" % i, [128, 512], F32)) for i in range(8)]

        def pbf(i):
            return pb[i][:].bitcast(BF16)

        def sbuf(st, name, shape, dt):
            return st.enter_context(nc.sbuf_tensor("sb_" + name, list(shape), dt))

        ident_f = sbuf(top, "ident_f", [128, 128], F32)
        ident_b = sbuf(top, "ident_b", [128, 128], BF16)
        junk = sbuf(top, "junk", [128, 128], BF16)
        S.op("sp", lambda e: e.dma_start(out=ident_f[:], in_=cd["ident_f"]), writes=["ident_f"], dma="c0")
        S.op("sp", lambda e: e.dma_start(out=ident_b[:], in_=cd["ident_b"]), writes=["ident_b"], dma="c1")

        def rms_prep(xt_ap, g_bc, xs_out, tag, xkey, reads_extra=(), xskey=None):
            ss = small[tag + "_ss"]
            xskey = xskey or (tag + "_xs")
            S.op("act", lambda e: e.activation(out=xs_out, in_=xt_ap, func=AF.Square, accum_out=ss[:]),
                 reads=[xkey], writes=[xskey, tag + "_ss"])
            S.op("act", lambda e: e.activation(out=ss[:], in_=ss[:], func=AF.Ln, scale=1.0 / D, bias=1e-6),
                 reads=[tag + "_ss"], writes=[tag + "_ss"])
            S.op("act", lambda e: e.activation(out=ss[:], in_=ss[:], func=AF.Exp, scale=-0.5), reads=[tag + "_ss"], writes=[tag + "_ss"])
            S.op("dve", lambda e: e.scalar_tensor_tensor(out=xs_out, in0=xt_ap, scalar=ss[:], in1=g_bc, op0=ALU.mult,
                                                          op1=ALU.mult),
                 reads=[xkey, tag + "_ss"] + list(reads_extra), writes=[xskey])

        small = {}

        def transpose8(xs_ap, dst_ap, skey, dkey, bank=0):
            pv = pbf(bank)
            bk = "pb%d" % bank
            for c in range(8):
                S.op("pe", lambda e, c=c: e.transpose(out=pv[:, c * 128:(c + 1) * 128], in_=xs_ap[:, c * 128:(c + 1) * 128],
                                                      identity=ident_b[:]),
                     reads=[skey, "ident_b"], writes=[bk])
            evac(alt(), dst_ap, pv.rearrange("p (c t) -> p c t", c=8), reads=[bk], writes=[dkey])

        if "A" in phases:
            with contextlib.ExitStack() as pa:
                A = lambda name, shape, dt: sbuf(pa, name, shape, dt)
                w_in = A("w_in", [128, 8, 3104], BF16)
                gpre = A("gpre", [128, D], F32)
                w1 = [A("w1k", [128, 16, 256], BF16), A("w1v", [128, 16, 256], BF16)]
                w2k = A("w2k", [128, 2, 128], BF16)
                w2v = A("w2v", [128, 2, 64], BF16)
                pos = [A("posk", [128, 16], BF16), A("posv", [128, 16], BF16)]
                c1 = [A("c1k", [128, 2], F32), A("c1v", [128, 2], F32)]
                anti_b = A("anti_b", [128, 128], BF16)
                d4 = A("d4", [128, 128], BF16)
                shiftw = A("shiftw", [17, 384], BF16)
                vm_tab = A("vm_tab", [128, 126], F32)
                add_tab = A("add_tab", [128, 126], F32)
                tri_ml = A("tri_ml", [128, 64], F32)
                resetmask = A("resetmask", [4, 512], F32)
                negreset = A("negreset", [4, 512], F32)
                selbc = A("selbc", [4, 4, 64], BF16)
                gRAb = A("gRAb", [4, 8, 2, 64], BF16)
                selpair = A("selpair", [4, 2, 128], F32)
                relbT = A("relbT", [33, 8], F32)
                G0 = A("G0", [128, 8, 128], BF16)
                G1 = A("G1", [128, 8, 128], BF16)
                bandx = A("bandx", [17, 8, 128], BF16)
                convw = A("convw", [128, 4, 4], F32)
                convb = A("convb", [128, 4], F32)
                gb = A("gb", [4, 2], F32)
                ngb1 = A("ngb1", [4, 1], F32)
                mln = A("mln", [128, 512], F32)
                kTs = [A("kTs0", [128, S_TOK], BF16), A("kTs1", [128, S_TOK], BF16)]
                kT_win = A("kT_win", [128, S_TOK], BF16)
                V_slc = A("V_slc", [128, NT, 2, 65], BF16)
                V_win = A("V_win", [128, NT, 2, 65], BF16)
                k_cmpT = A("k_cmpT", [128, 256], BF16)
                hidT_v = A("hidT_v", [128, 2, 2, 256], BF16)
                hid_k = A("hid_k", [128, 2, 2, 32], BF16)
                vcmp = A("vcmp", [128, 2, 2, 129], BF16)
                xt = [A("xt0", [128, D], F32)] * 2
                xs = A("xs", [128, D], BF16)
                hT = A("hT", [128, 8, 512], BF16)
                qTz = [A("qTz0", [128, 4, 512], BF16), A("qTz1", [128, 4, 512], BF16)]
                kc2 = [[A("kc2_%d%d" % (kv, h), [128, 528], BF16) for h in range(2)] for kv in range(2)]
                uT = A("uT", [128, 4, 515], BF16)
                qkml = A("qkml", [128, 4, 512], BF16)
                k_tm = A("k_tm", [128, 4, 256], BF16)
                v_ml = A("v_ml", [128, 4, 4, 129], BF16)
                og_n = A("og_n", [128, 4, 512], BF16)
                gates = A("gates", [128, 4, 24], F32)
                gI = A("gI", [4, 512], F32)
                gL = A("gL", [4, 512], F32)
                gG = A("gG", [4, 512], F32)
                gRA = A("gRA", [4, 8, 2, 64], F32)
                gE3 = A("gE3", [4, 512], F32)
                gsm = A("gsm", [4, 64], F32)
                E3_tm = A("E3_tm", [128, 4, 4], F32)
                es_tm = A("es_tm", [128, 4, 4], F32)
                decay_bc = A("decay_bc", [128, 2, 8], F32)
                Cst = A("Cst", [128, 2, 129], F32)
                qba = A("qba", [128, 2, 64], F32)
                qbb = A("qbb", [128, 64], BF16)
                STb = A("STb", [128, 64], BF16)
                kw = A("kw", [128, 256], BF16)
                hh = A("hh", [128, 4, 128], F32)
                cacc = hh[:].rearrange("p h d -> p (h d)")
                mls = A("mls", [128, 16], F32)
                Eb = [A("Eb0", [128, 4, 128], BF16), A("Eb1", [128, 4, 128], BF16), A("Eb2", [128, 4, 128], BF16)]
                zrow = A("zrow", [1, 512], BF16)
                asm = A("asm", [128, 32], F32)
                imp = A("imp", [128, 64], F32)
                sc = A("sc", [128, 64], F32)
                sc2 = imp
                m8 = A("m8", [128, 16], F32)
                negsel = A("negsel", [128, 64], BF16)
                qTs = [A("qTs0", [128, 4, 128], BF16), A("qTs1", [128, 4, 128], BF16)]
                cmp_sb = A("cmp_sb", [128, 516], F32)
                ytmp = cmp_sb[:, 0:256].rearrange("p (g d) -> p g d", g=4)
                yacc = A("yacc", [128, 512], F32)
                ycat = [A("ycat0", [128, D], BF16)] * 2
                ycf = ycat[0][:].bitcast(F32)
                onehot = ycf[0:33, 0:384]
                Fsb = ycf[64:72, 0:384]
                for nm in ("pre_ss",):
                    small[nm] = A(nm, [128, 1], F32)

                def ld(dst, src, key, sem, eng="sp"):
                    S.op(eng, lambda e: e.dma_start(out=dst, in_=src), writes=[key], dma=sem)

                for c in range(8):
                    ld(w_in[:, c, :], w_in_d[c * 128:(c + 1) * 128, :], "w_in", "win%d" % c, eng="pool")
                ld(gpre[:], gpre_d.partition_broadcast(128), "gpre", "c2")
                ld(w1[0][:], w1k_d.rearrange("(j p) n -> p j n", p=128), "w1k", "c3", eng="pool")
                ld(w1[1][:], w1v_d.rearrange("(j p) n -> p j n", p=128), "w1v", "c4", eng="pool")
                ld(w2k[:, :, 0:64], w2k_d.rearrange("(c p) n -> p c n", p=128), "w2k", "c5", eng="pool")
                ld(w2k[:, :, 64:128], w2k_d.rearrange("(c p) n -> p c n", p=128), "w2k", "c6", eng="pool")
                ld(w2v[:], w2v_d.rearrange("(c p) n -> p c n", p=128), "w2v", "c7", eng="pool")
                ld(pos[0][:], posk_d, "posk", "c8", eng="pool")
                ld(pos[1][:], posv_d, "posv", "c9", eng="pool")
                for (t_, nm) in ((anti_b, "anti_b"), (d4, "d4"), (shiftw, "shiftw"), (vm_tab, "vm_tab"),
                                 (add_tab, "add_tab"), (tri_ml, "tri_ml"), (resetmask, "resetmask"),
                                 (negreset, "negreset"), (selbc, "selbc"), (selpair, "selpair"),
                                 (vcmp, "vcmp_init")):
                    key = "vcmp" if nm == "vcmp_init" else nm
                    ld(t_[:], cd[nm], key, "k_" + nm)
                ld(relbT[:], relb_d, "relbT", "c10")
                ld(onehot, cd["onehot"], "ycat0", "k_onehot")
                ld(kTs[0][64:128, :], cd["expand"][:, 0:S_TOK], "kTs_e0", "c15")
                ld(kTs[1][0:64, :], cd["expand"][:, 0:S_TOK], "kTs_e1", "c16")
                ld(convw[:], convw_d, "convw", "c11")
                ld(convb[:], convb_d, "convb", "c12")
                ld(gb[:], gb_d, "gb", "c13")
                ld(mln[:], mln_d.partition_broadcast(128), "mln", "c14")

                S.op("pool", lambda e: e.memset(V_slc[:], 1.0), writes=["V_slc"])
                S.op("pool", lambda e: e.memset(V_win[:], 1.0), writes=["V_win"])
                S.op("pool", lambda e: e.memset(v_ml[:], 1.0), writes=["v_ml"])
                S.op("pool", lambda e: e.memset(hidT_v[:], 0.0), writes=["hidT_v"])
                S.op("pool", lambda e: e.memset(k_cmpT[:], 0.0), writes=["k_cmpT"])
                S.op("pool", lambda e: e.memset(uT[:], 0.0), writes=["uT"])
                S.op("pool", lambda e: e.memset(Cst[:], 0.0), writes=["Cst"])
                S.op("pool", lambda e: e.memset(gsm[:], 0.0), writes=["gsm"])
                S.op("pool", lambda e: e.memset(zrow[:], 0.0), writes=["zrow"])
                if FILLERS:
                    zfill = A("zfill", [128, 256], BF16)
                    zi = S.op("pool", lambda e: e.memset(zfill[:], 0.0), writes=["zfill"])
                    S.filler = (0, (lambda e: e.matmul(pb[7][:, 0:256], lhsT=zfill[:, 0:128], rhs=zfill[:, 0:256], start=True, stop=True)), zi)
                S.op("pool", lambda e: e.memset(qTz[0][:], 0.0), writes=["qT"])
                S.op("pool", lambda e: e.memset(qTz[1][:], 0.0), writes=["qT"])
                for kv in range(2):
                    for h in range(2):
                        S.op("pool", lambda e, kv=kv, h=h: e.memset(kc2[kv][h][:], 0.0), writes=["kc2_%d%d" % (kv, h)])
                S.op("dve", lambda e: e.tensor_scalar_mul(out=ngb1[:], in0=gb[:, 1:2], scalar1=-1.0), reads=["gb"], writes=["ngb1"])

                hTf = hT[:].rearrange("p a b -> p (a b)").bitcast(F32)
                G0f = hTf[:, 0:1024].rearrange("p (h t) -> p h t", h=8)
                bandf = hTf[0:17, 1024:2048].rearrange("p (h t) -> p h t", h=8)
                mm(pb[1][64:72, 0:384], relbT[:], onehot, True, True, ["relbT", "ycat0"], ["pb1"])
                evac("dve", Fsb, pb[1][64:72, 0:384], ["pb1"], ["ycat0"])
                S.op("sp", lambda e: e.dma_start(out=F_d, in_=Fsb), reads=["ycat0"], writes=["F_d"], dma="fst")
                Ft = F_d.tensor
                S.op("sp", lambda e: e.dma_start(out=G0f, in_=bass.AP(tensor=Ft, offset=0, ap=[[1, 128], [384, 8], [1, 128]])),
                     reads=["F_d"], writes=["hT"], dma="fl0")
                S.op("dve", lambda e: e.tensor_copy(out=G0[:], in_=G0f), reads=["hT"], writes=["G0"])
                S.op("sp", lambda e: e.dma_start(out=G0f, in_=bass.AP(tensor=Ft, offset=128, ap=[[1, 128], [384, 8], [1, 128]])),
                     reads=["F_d"], writes=["hT"], dma="fl1")
                S.op("dve", lambda e: e.tensor_copy(out=G1[:], in_=G0f), reads=["hT"], writes=["G1"])
                S.op("pool", lambda e: e.memset(bandf, NEG), writes=["hT"])
                S.op("sp", lambda e: e.dma_start(out=bandf[0:16, :, :], in_=bass.AP(tensor=Ft, offset=0, ap=[[16, 16], [384, 8], [1, 128]])),
                     reads=["F_d"], writes=["hT"], dma="fl2")
                S.op("dve", lambda e: e.tensor_copy(out=bandx[:], in_=bandf), reads=["hT"], writes=["bandx"])

                for kv in range(2):
                    for hc in range(2):
                        for jp in range(16):
                            mm(pb[1][:, hc:hc + 1], w1[kv][:, jp, hc * 128:(hc + 1) * 128], pos[kv][:, jp:jp + 1],
                               jp == 0, jp == 15, ["w1k" if kv == 0 else "w1v", "posk" if kv == 0 else "posv"], ["pb1"])
                    evac("dve", c1[kv][:], pb[1][:, 0:2], ["pb1"], ["c1_%d" % kv])

                FMQ, FMKC, FMVC, FMKS, FMKW, FMMQ, FMMK, FMMI, FMMF = 0, 512, 768, 1024, 1152, 1280, 1536, 1792, 1796
                TM0, TMV, TMO = 1800, 2080, 2592
                pj = [1]

                def pjbank():
                    pj[0] = 3 - pj[0]
                    return pj[0]

                def proj_fm(lhs_fn, M):
                    bi = pjbank()
                    for k in range(8):
                        mm(pb[bi][0:M, :], lhs_fn(k), hT[:, k, :], k == 0, k == 7, ["w_in", "hT"], ["pb%d" % bi])
                    return bi

                for st in range(NST):
                    for tt in range(4):
                        n = 4 * st + tt
                        sl = 0
                        S.op("sp", lambda e, n=n, sl=sl: e.dma_start(out=xt[sl][:], in_=x_d[n * 128:(n + 1) * 128, :]),
                             writes=["xt%d" % sl], dma="x%d" % sl)
                        rms_prep(xt[sl][:], gpre[:], xs[:], "pre", "xt%d" % sl, reads_extra=["gpre"])
                        transpose8(xs[:], hT[:, :, tt * 128:(tt + 1) * 128], "pre_xs", "hT", bank=pjbank())

                    for g in range(4):
                        bi = proj_fm(lambda k, g=g: w_in[:, k, FMQ + 128 * g:FMQ + 128 * (g + 1)], 128)
                        evac("act", qTz[0][0:64, g, :], pb[bi][0:64, :], ["pb%d" % bi], ["qT"], scale=0.125)
                        evac("dve", qTz[1][64:128, g, :], pb[bi][64:128, :], ["pb%d" % bi], ["qT"], scale=0.125)
                    for kv in range(2):
                        for h in range(2):
                            base = (FMKC if kv == 0 else FMVC) + 128 * h
                            key = "kc2_%d%d" % (kv, h)
                            buf = kc2[kv][h]
                            if st > 0:
                                S.op("pool", lambda e, buf=buf: e.tensor_copy(out=buf[:, 0:16], in_=buf[:, 512:528]),
                                     reads=[key], writes=[key])
                            bi = proj_fm(lambda k, base=base: w_in[:, k, base:base + 128], 128)
                            evac("act", buf[0:64, 16:528], pb[bi][0:64, :], ["pb%d" % bi], [key])
                            evac("dve", buf[64:128, 15:527], pb[bi][64:128, :], ["pb%d" % bi], [key])
                    bi = proj_fm(lambda k: w_in[:, k, FMKS:FMKS + 128], 128)
                    evac("act", kTs[0][0:64, st * 512:(st + 1) * 512], pb[bi][0:64, :], ["pb%d" % bi], [("kslc", st)])
                    evac("dve", kTs[1][64:128, st * 512:(st + 1) * 512], pb[bi][64:128, :], ["pb%d" % bi], [("kslc", st)])
                    bi = proj_fm(lambda k: w_in[:, k, FMKW:FMKW + 128], 128)
                    evac(alt(), kT_win[:, st * 512:(st + 1) * 512], pb[bi][:, :], ["pb%d" % bi], [("kwin", st)])
                    if st > 0:
                        S.op("pool", lambda e: e.tensor_copy(out=uT[:, :, 0:3], in_=uT[:, :, 512:515]), reads=["uT"], writes=["uT"])
                    for c in range(4):
                        base = FMMQ + 128 * c
                        bi = proj_fm(lambda k, base=base: w_in[:, k, base:base + 128], 128)
                        evac(alt(), uT[:, c, 3:515], pb[bi][:, :], ["pb%d" % bi], ["uT"])
                    bi = proj_fm(lambda k: w_in[:, k, FMMI:FMMI + 4], 4)
                    S.op("act", lambda e, bi=bi: e.activation(out=gI[:], in_=pb[bi][0:4, :], func=AF.Identity, bias=gb[:, 0:1]),
                         reads=["pb%d" % bi, "gb"], writes=["gI"])
                    bi = proj_fm(lambda k: w_in[:, k, FMMF:FMMF + 4], 4)
                    S.op("act", lambda e, bi=bi: e.activation(out=gE3[:], in_=pb[bi][0:4, :], func=AF.Exp, scale=-1.0, bias=ngb1[:]),
                         reads=["pb%d" % bi, "ngb1"], writes=["gE3"])
                    S.op("act", lambda e: e.activation(out=gE3[:], in_=gE3[:], func=AF.Ln, bias=1.0), reads=["gE3"], writes=["gE3"])
                    S.op("dve", lambda e: e.tensor_tensor_scan(out=gL[:], data0=resetmask[:], data1=gE3[:], initial=0.0,
                                                               op0=ALU.mult, op1=ALU.add),
                         reads=["gE3", "resetmask"], writes=["gL"])
                    S.op("dve", lambda e: e.tensor_add(out=gI[:], in0=gI[:], in1=gL[:]), reads=["gI", "gL"], writes=["gI"])
                    S.op("dve", lambda e: e.tensor_tensor_scan(out=gG[:], data0=negreset[:], data1=gI[:], initial=-1.0e30,
                                                               op0=ALU.add, op1=ALU.max),
                         reads=["gI", "negreset"], writes=["gG"])
                    bLv, GbLv, mnx, mst, Mend, dec, carry = (gsm[:, 0:8], gsm[:, 8:16], gsm[:, 16:24], gsm[:, 24:32],
                                                             gsm[:, 32:40], gsm[:, 40:48], gsm[:, 48:49])
                    S.op("dve", lambda e: e.tensor_scalar_mul(out=bLv, in0=gL[:, 63:512:64], scalar1=-1.0), reads=["gL"], writes=["gsm"])
                    S.op("dve", lambda e: e.tensor_add(out=GbLv, in0=gG[:, 63:512:64], in1=bLv), reads=["gG", "gsm"], writes=["gsm"])
                    S.op("dve", lambda e: e.tensor_tensor_scan(out=mnx, data0=bLv, data1=GbLv, initial=carry, op0=ALU.add, op1=ALU.max),
                         reads=["gsm"], writes=["gsm"])
                    S.op("dve", lambda e: e.tensor_copy(out=gsm[:, 24:25], in_=carry), reads=["gsm"], writes=["gsm"])
                    S.op("dve", lambda e: e.tensor_copy(out=gsm[:, 25:32], in_=gsm[:, 16:23]), reads=["gsm"], writes=["gsm"])
                    S.op("dve", lambda e: e.tensor_copy(out=carry, in_=gsm[:, 23:24]), reads=["gsm"], writes=["gsm"])
                    S.op("dve", lambda e: e.tensor_max(out=Mend, in0=mst, in1=gG[:, 63:512:64]), reads=["gsm", "gG"], writes=["gsm"])
                    S.op("dve", lambda e: e.tensor_sub(out=dec, in0=mst, in1=Mend), reads=["gsm"], writes=["gsm"])
                    S.op("act", lambda e: e.activation(out=dec, in_=dec, func=AF.Exp), reads=["gsm"], writes=["gsm"])
                    mst_bc = mst.unsqueeze(2).to_broadcast([4, 8, 64])
                    Mend_bc = Mend.unsqueeze(2).to_broadcast([4, 8, 64])
                    v3 = lambda t_: t_[:].rearrange("p (c t) -> p c t", c=8)
                    S.op("dve", lambda e: e.tensor_max(out=v3(gG), in0=v3(gG), in1=mst_bc), reads=["gG", "gsm"], writes=["gG"])
                    S.op("dve", lambda e: e.tensor_sub(out=gRA[:, :, 0, :], in0=Mend_bc, in1=v3(gG)), reads=["gG", "gsm"], writes=["gRA"])
                    S.op("dve", lambda e: e.tensor_sub(out=gRA[:, :, 1, :], in0=mst_bc, in1=v3(gG)), reads=["gG", "gsm"], writes=["gRA"])
                    S.op("act", lambda e: e.activation(out=gRAb[:], in_=gRA[:], func=AF.Exp), reads=["gRA"], writes=["gRAb"])
                    S.op("dve", lambda e: e.tensor_sub(out=gE3[:], in0=gL[:], in1=gG[:]), reads=["gL", "gG"], writes=["gE3"])
                    S.op("act", lambda e: e.activation(out=gE3[:], in_=gE3[:], func=AF.Exp), reads=["gE3"], writes=["gE3"])
                    S.op("dve", lambda e: e.tensor_sub(out=v3(gI), in0=v3(gI), in1=Mend_bc), reads=["gI", "gsm"], writes=["gI"])
                    S.op("act", lambda e: e.activation(out=gI[:], in_=gI[:], func=AF.Exp), reads=["gI"], writes=["gI"])
                    S.op("dve", lambda e: e.tensor_scalar_mul(out=gI[:], in0=gI[:], scalar1=0.125), reads=["gI"], writes=["gI"])
                    for tt in range(4):
                        S.op("pe", lambda e, tt=tt: e.transpose(out=pb[0][:, 0:4], in_=gE3[:, tt * 128:(tt + 1) * 128], identity=ident_f[0:4, 0:4]),
                             reads=["gE3", "ident_f"], writes=["pb0"], fence=True)
                        evac("dve", E3_tm[:, tt, :], pb[0][:, 0:4], ["pb0"], ["E3_tm"])
                        S.op("pe", lambda e, tt=tt: e.transpose(out=pb[0][:, 0:4], in_=gI[:, tt * 128:(tt + 1) * 128], identity=ident_f[0:4, 0:4]),
                             reads=["gI", "ident_f"], writes=["pb0"], fence=True)
                        evac("dve", es_tm[:, tt, :], pb[0][:, 0:4], ["pb0"], ["es_tm"])
                    for cp in range(2):
                        mm(pb[0][:, 0:8], selpair[:, cp, :], dec, True, True, ["selpair", "gsm"], ["pb0"], fence=True)
                        evac("dve", decay_bc[:, cp, :], pb[0][:, 0:8], ["pb0"], ["decay_bc"])

                    for c in range(4):
                        S.op("dve", lambda e, c=c: e.tensor_scalar_mul(out=cacc, in0=uT[:, c, 3:515], scalar1=convw[:, c, 3:4]),
                             reads=["uT", "convw"], writes=["hh"])
                        for j in range(3):
                            S.op("dve", lambda e, c=c, j=j: e.scalar_tensor_tensor(out=cacc, in0=uT[:, c, j:j + 512], scalar=convw[:, c, j:j + 1],
                                                                                   in1=cacc, op0=ALU.mult, op1=ALU.add),
                                 reads=["uT", "convw", "hh"], writes=["hh"])
                        S.op("act", lambda e, c=c: e.activation(out=qkml[:, c, :], in_=cacc, func=AF.Silu, bias=convb[:, c:c + 1]),
                             reads=["hh", "convb"], writes=["qkml"])
                    for tt in range(4):
                        bk_ = pjbank()
                        pv0 = pbf(bk_)
                        for c in range(2):
                            S.op("pe", lambda e, tt=tt, c=c, pv0=pv0: e.transpose(out=pv0[:, c * 128:(c + 1) * 128], in_=qkml[:, 2 + c, tt * 128:(tt + 1) * 128],
                                                                                  identity=ident_b[:]),
                                 reads=["qkml", "ident_b"], writes=["pb%d" % bk_])
                        evac(alt(), k_tm[:, tt, :], pv0[:, 0:256], ["pb%d" % bk_], ["k_tm"])

                    for tt in range(4):
                        n = 4 * st + tt
                        tsl = slice(tt * 128, (tt + 1) * 128)
                        bi = pjbank()
                        for k in range(8):
                            mm(pb[bi][:, 0:280], hT[:, k, tsl], w_in[:, k, TM0:TM0 + 280], k == 0, k == 7, ["w_in", "hT"], ["pb%d" % bi])
                        evac("act", V_slc[:, n, :, 0:64], pb[bi][:, 0:128].rearrange("p (h d) -> p h d", h=2), ["pb%d" % bi], [("vslc", n), "V_slc"])
                        evac("dve", V_win[:, n, :, 0:64], pb[bi][:, 128:256].rearrange("p (h d) -> p h d", h=2), ["pb%d" % bi], [("vwin", n), "V_win"])
                        S.op("act", lambda e, bi=bi, tt=tt: e.activation(out=gates[:, tt, :], in_=pb[bi][:, 256:280], func=AF.Sigmoid),
                             reads=["pb%d" % bi], writes=["gates"])
                        bi = pjbank()
                        for k in range(8):
                            mm(pb[bi][:, :], hT[:, k, tsl], w_in[:, k, TMV:TMV + 512], k == 0, k == 7, ["w_in", "hT"], ["pb%d" % bi])
                        evac("dve", v_ml[:, tt, :, 0:128], pb[bi][:, :].rearrange("p (h d) -> p h d", h=4), ["pb%d" % bi], ["v_ml"])
                        bi = pjbank()
                        for k in range(8):
                            mm(pb[bi][:, :], hT[:, k, tsl], w_in[:, k, TMO:TMO + 512], k == 0, k == 7, ["w_in", "hT"], ["pb%d" % bi])
                        S.op("act", lambda e, bi=bi, tt=tt: e.activation(out=og_n[:, tt, :], in_=pb[bi][:, :], func=AF.Sigmoid),
                             reads=["pb%d" % bi], writes=["og_n"])
                        S.op("pool", lambda e, tt=tt: e.tensor_mul(out=og_n[:, tt, :], in0=og_n[:, tt, :], in1=mln[:]),
                             reads=["og_n", "mln"], writes=["og_n"])

                    r0 = 1 if st == 0 else 0
                    nr_ = 32 - r0
                    i0 = 32 * st - 1 + r0
                    for kv in range(2):
                        for h in range(2):
                            key = "kc2_%d%d" % (kv, h)
                            for hc in range(2):
                                for jp in range(16):
                                    c0_ = 2 * jp + 16 * r0
                                    mm(pb[0][:, 0:nr_], w1[kv][:, jp, hc * 128:(hc + 1) * 128],
                                       kc2[kv][h][:, c0_:c0_ + 16 * (nr_ - 1) + 1:16], jp == 0, jp == 15,
                                       ["w1k" if kv == 0 else "w1v", key], ["pb0"])
                                dst = hid_k[:, h, hc, 0:nr_] if kv == 0 else hidT_v[:, h, hc, i0:i0 + nr_]
                                S.op("act", lambda e, dst=dst, kv=kv, hc=hc: e.activation(out=dst, in_=pb[0][:, 0:nr_], func=AF.Silu,
                                                                                       bias=c1[kv][:, hc:hc + 1]),
                                     reads=["pb0", "c1_%d" % kv], writes=["hid_k" if kv == 0 else "hidT_v"])
                    for h in range(2):
                        for hc in range(2):
                            mm(pb[0][:, 0:nr_], w2k[:, hc, :], hid_k[:, h, hc, 0:nr_], hc == 0, hc == 1, ["w2k", "hid_k"], ["pb0"])
                        evac("dve", k_cmpT[64 * h:64 * h + 64, i0:i0 + nr_], pb[0][64 * h:64 * h + 64, 0:nr_], ["pb0"], ["k_cmpT"])
                        for ch in sorted(set([max(i0, 0) // 128, (i0 + nr_ - 1) // 128])):
                            for hc in range(2):
                                mm(pb[0][:, 0:64], hidT_v[:, h, hc, ch * 128:(ch + 1) * 128], w2v[:, hc, :], hc == 0, hc == 1,
                                   ["hidT_v", "w2v"], ["pb0"])
                            evac("act", vcmp[:, h, ch, 0:64], pb[0][:, 0:64], ["pb0"], ["vcmp"])

                    for tt in range(4):
                        n = 4 * st + tt
                        tsl = slice(tt * 128, (tt + 1) * 128)
                        ysl = 0
                        sbk = [0]

                        def sbank():
                            sbk[0] = (sbk[0] + 1) % 3
                            return (3, 4, 6)[sbk[0]]

                        def zero_bank(bi, ncol):
                            mm(pb[bi][:, 0:ncol], zrow[0:1, 0:128], zrow[0:1, 0:ncol], True, False, ["zrow"], ["pb%d" % bi], skip=True)

                        ebk = [0]

                        def attn_chunk(kT_ap, kkeys, q_ap, extras, pv_fn, last, bank=None):
                            bi = sbank() if bank is None else bank
                            mm(pb[bi][:, :], kT_ap, q_ap, True, len(extras) == 0, kkeys + ["qT"], ["pb%d" % bi])
                            for i_, (l_, r_, ks_) in enumerate(extras):
                                mm(pb[bi][:, :], l_, r_, False, i_ == len(extras) - 1, ks_, ["pb%d" % bi])
                            ebk[0] = (ebk[0] + 1) % 3
                            E = Eb[ebk[0]]
                            S.op("act", lambda e, bi=bi, E=E: e.activation(out=E[:].rearrange("p g t -> p (g t)"), in_=pb[bi][:, :], func=AF.Exp),
                                 reads=["pb%d" % bi], writes=["Eb%d" % ebk[0]])
                            for g in range(4):
                                o_ap, v_ap, oks, vks = pv_fn(g)
                                mm(o_ap, E[:, g, :], v_ap, False, last, ["Eb%d" % ebk[0]] + vks, oks, skip=True)

                        for kvh in range(2):
                            hb = 64 * kvh
                            q_ap = qTz[kvh][:, :, tsl]
                            idb4 = ident_b[:].unsqueeze(1).to_broadcast([128, 4, 128])
                            val = 8 * n + 6
                            chunks = [0] + ([1] if val >= 128 else [])
                            zero_bank(5, 258)
                            zero_bank(6, 258)

                            def pv_cmp(g, ch=None):
                                bi = 5 + g // 2
                                return (pb[bi][:, (g % 2) * 129:(g % 2) * 129 + 129], None, ["pb%d" % bi], None)

                            for ci, ch in enumerate(chunks):
                                v_ = val - 128 * ch
                                extras = []
                                if v_ - 15 <= 127:
                                    off = 254 - v_
                                    extras.append((shiftw[:, off:off + 128], bandx[:, 4 * kvh:4 * kvh + 4, :], ["shiftw", "bandx"]))

                                def pvf(g, ch=ch):
                                    bi = 5 + g // 2
                                    c0_ = (g % 2) * 129
                                    return (pb[bi][:, c0_:c0_ + 129], vcmp[:, kvh, ch, :], ["pb%d" % bi], ["vcmp"])

                                attn_chunk(k_cmpT[:, ch * 128:(ch + 1) * 128], ["k_cmpT"], q_ap, extras, pvf, ci == len(chunks) - 1, bank=3 + ci)
                            for b2 in range(2):
                                S.op("act", lambda e, b2=b2: e.copy(out=cmp_sb[:, 258 * b2:258 * b2 + 258], in_=pb[5 + b2][:, 0:258]),
                                     reads=["pb%d" % (5 + b2)], writes=["cmp_sb"])
                            Ov = [cmp_sb[:, 0:258].rearrange("p (g c) -> p g c", g=2), cmp_sb[:, 258:516].rearrange("p (g c) -> p g c", g=2)]
                            for b2 in range(2):
                                S.op("dve", lambda e, b2=b2: e.tensor_scalar_max(out=asm[:, 2 * b2:2 * b2 + 2], in0=Ov[b2][:, :, 64], scalar1=1e-30),
                                     reads=["cmp_sb"], writes=["asm"])
                            S.op("dve", lambda e: e.reciprocal(out=asm[:, 0:4], in_=asm[:, 0:4]), reads=["asm"], writes=["asm"])
                            gsl = gates[:, tt, 12 * kvh:12 * kvh + 12]
                            S.op("dve", lambda e, gsl=gsl: e.tensor_mul(out=asm[:, 4:8], in0=asm[:, 0:4], in1=gsl[:, 0:12:3]), reads=["asm", "gates"], writes=["asm"])
                            yv = yacc[:, 256 * kvh:256 * kvh + 256].rearrange("p (g d) -> p g d", g=4)
                            for b2 in range(2):
                                S.op("dve", lambda e, b2=b2: e.tensor_mul(out=yv[:, 2 * b2:2 * b2 + 2, :], in0=Ov[b2][:, :, 0:64],
                                                                          in1=asm[:, 4 + 2 * b2:6 + 2 * b2].unsqueeze(2).to_broadcast([128, 2, 64])),
                                     reads=["cmp_sb", "asm"], writes=["yacc"])
                            for g in range(4):
                                src = Ov[g // 2][:, g % 2, 65:129]
                                if g == 0:
                                    S.op("dve", lambda e, src=src: e.tensor_scalar_mul(out=imp[:], in0=src, scalar1=asm[:, 0:1]),
                                         reads=["cmp_sb", "asm"], writes=["imp"])
                                else:
                                    S.op("dve", lambda e, src=src, g=g: e.scalar_tensor_tensor(out=imp[:], in0=src, scalar=asm[:, g:g + 1], in1=imp[:],
                                                                                               op0=ALU.mult, op1=ALU.add),
                                         reads=["cmp_sb", "asm", "imp"], writes=["imp"])
                            def combine(br, bi):
                                O4 = pb[bi][:, 0:260].rearrange("p (g c) -> p g c", g=4)
                                S.op("dve", lambda e, O4=O4: e.tensor_scalar_max(out=asm[:, 8:12], in0=O4[:, :, 64], scalar1=1e-30),
                                     reads=["pb%d" % bi], writes=["asm"])
                                S.op("dve", lambda e: e.reciprocal(out=asm[:, 8:12], in_=asm[:, 8:12]), reads=["asm"], writes=["asm"])
                                S.op("dve", lambda e, gsl=gsl, br=br: e.tensor_mul(out=asm[:, 12:16], in0=asm[:, 8:12], in1=gsl[:, br:12:3]),
                                     reads=["asm", "gates"], writes=["asm"])
                                S.op("dve", lambda e, O4=O4: e.tensor_mul(out=ytmp, in0=O4[:, :, 0:64],
                                                                         in1=asm[:, 12:16].unsqueeze(2).to_broadcast([128, 4, 64])),
                                     reads=["pb%d" % bi, "asm"], writes=["cmp_sb"])
                                S.op("pool", lambda e, yv=yv: e.tensor_add(out=yv, in0=yv, in1=ytmp), reads=["cmp_sb", "yacc"], writes=["yacc"])
                            zero_bank(5, 260)

                            def pv_win(g, kc):
                                return (pb[5][:, g * 65:g * 65 + 65], V_win[:, kc, kvh, :], ["pb5"], [("vwin", kc)])

                            for kc in range(max(0, n - 4), n + 1):
                                extras = []
                                if kc == n:
                                    extras.append((anti_b[:], G0[:, 4 * kvh:4 * kvh + 4, :], ["anti_b", "G0"]))
                                elif kc == n - 1:
                                    extras.append((anti_b[:], G1[:, 4 * kvh:4 * kvh + 4, :], ["anti_b", "G1"]))
                                elif kc == n - 4:
                                    extras.append((ident_b[:], d4[:].unsqueeze(1).to_broadcast([128, 4, 128]), ["ident_b", "d4"]))
                                attn_chunk(kT_win[:, kc * 128:(kc + 1) * 128], [("kwin", kc // 4)], q_ap, extras,
                                           lambda g, kc=kc: pv_win(g, kc), kc == n)
                            combine(2, 5)
                            w0 = 62 - 2 * n
                            S.op("dve", lambda e, w0=w0: e.tensor_mul(out=sc[:], in0=imp[:], in1=vm_tab[:, w0:w0 + 64]), reads=["imp", "vm_tab"], writes=["sc"])
                            S.op("dve", lambda e, w0=w0: e.tensor_add(out=sc[:], in0=sc[:], in1=add_tab[:, w0:w0 + 64]), reads=["sc", "add_tab"], writes=["sc"])
                            S.op("dve", lambda e: e.memset(sc[:, 0:1], 1.0e4), reads=["sc"], writes=["sc"])
                            S.op("dve", lambda e: e.max(out=m8[:, 0:8], in_=sc[:]), reads=["sc"], writes=["m8"])
                            S.op("dve", lambda e: e.match_replace(out=sc2[:], in_to_replace=m8[:, 0:8], in_values=sc[:], imm_value=-2.0),
                                 reads=["sc", "m8"], writes=["imp"])
                            S.op("dve", lambda e: e.max(out=m8[:, 8:16], in_=sc2[:]), reads=["imp"], writes=["m8"])
                            S.op("dve", lambda e: e.tensor_scalar(out=negsel[:], in0=sc[:], scalar1=m8[:, 15:16], scalar2=NEG, op0=ALU.is_lt, op1=ALU.mult),
                                 reads=["sc", "m8"], writes=["negsel"])
                            oh = 64 * (1 - kvh)
                            nb_ = sbank()
                            S.op("pe", lambda e, oh=oh, nb_=nb_: e.transpose(out=pbf(nb_)[oh:oh + 64, 0:128], in_=negsel[:], identity=ident_b[:]),
                                 reads=["negsel", "ident_b"], writes=["pb%d" % nb_])
                            S.op("dve", lambda e, oh=oh, kvh=kvh, nb_=nb_: e.tensor_copy(out=qTs[kvh][oh:oh + 64, :, :],
                                                                                       in_=pbf(nb_)[oh:oh + 64, 0:128].unsqueeze(1).to_broadcast([64, 4, 128])),
                                 reads=["pb%d" % nb_], writes=["qTs%d" % kvh])
                            S.op("pool", lambda e, hb=hb, kvh=kvh, tsl=tsl: e.tensor_copy(out=qTs[kvh][hb:hb + 64, :, :], in_=qTz[kvh][hb:hb + 64, :, tsl]),
                                 reads=["qT"], writes=["qTs%d" % kvh])
                            zero_bank(5, 260)

                            def pv_sel(g, kc):
                                return (pb[5][:, g * 65:g * 65 + 65], V_slc[:, kc, kvh, :], ["pb5"], [("vslc", kc)])

                            for kc in range(n + 1):
                                extras = []
                                if kc == n:
                                    extras.append((anti_b[:], G0[:, 4 * kvh:4 * kvh + 4, :], ["anti_b", "G0"]))
                                else:
                                    if kc == n - 1:
                                        extras.append((anti_b[:], G1[:, 4 * kvh:4 * kvh + 4, :], ["anti_b", "G1"]))
                                attn_chunk(kTs[kvh][:, kc * 128:(kc + 1) * 128], [("kslc", kc // 4), "kTs_e%d" % kvh, "qTs%d" % kvh], qTs[kvh][:, :, :], extras,
                                           lambda g, kc=kc: pv_sel(g, kc), kc == n)
                            combine(1, 5)
                        S.op("act", lambda e, ysl=ysl: e.copy(out=ycat[ysl][:, 0:512], in_=yacc[:]), reads=["yacc"], writes=["ycat%d" % ysl])

                        for hp in range(2):
                            for cc in range(2):
                                cg = tt * 2 + cc
                                csl = slice(tt * 128 + cc * 64, tt * 128 + cc * 64 + 64)
                                pr = slice(64 * cc, 64 * cc + 64)
                                for hh_ in range(2):
                                    h = 2 * hp + hh_
                                    hbq = 64 * hh_
                                    hr = slice(hbq, hbq + 64)
                                    mm(pb[0][hr, 0:128], selbc[:, h, :], gRAb[:, cg, :, :].rearrange("p a t -> p (a t)"), True, True,
                                       ["selbc", "gRAb"], ["pb0"], fence=True)
                                    S.op("dve", lambda e, hr=hr, hp=hp, csl=csl: e.tensor_mul(
                                        out=qba[hr, :, :], in0=qkml[hr, hp, csl].unsqueeze(1).to_broadcast([64, 2, 64]),
                                        in1=pb[0][hr, 0:128].rearrange("p (a t) -> p a t", a=2)),
                                        reads=["pb0", "qkml"], writes=["qba"])
                                    S.op("act", lambda e, hr=hr: e.copy(out=qbb[hr, :], in_=qba[hr, 0, :]), reads=["qba"], writes=["qbb"])
                                    mm(pb[0][pr, 128:192], qkml[hr, 2 + hp, csl], qbb[hr, :], True, True, ["qkml", "qbb"], ["pb0"], fence=True)
                                    S.op("dve", lambda e, pr=pr, tt=tt, h=h: e.scalar_tensor_tensor(
                                        out=STb[pr, :], in0=pb[0][pr, 128:192], scalar=es_tm[pr, tt, h:h + 1], in1=tri_ml[pr, :],
                                        op0=ALU.mult, op1=ALU.mult),
                                        reads=["pb0", "es_tm", "tri_ml"], writes=["STb"])
                                    mm(pb[0][pr, 192 + hh_ * 129:192 + hh_ * 129 + 129], STb[pr, :], v_ml[pr, tt, h, :], True, False, ["STb", "v_ml"], ["pb0"], fence=True)
                                    mm(pb[0][pr, 192 + hh_ * 129:192 + hh_ * 129 + 129], qba[hr, 1, :], Cst[hr, hp, :], False, True, ["qba", "Cst"], ["pb0"], fence=True)
                                S.op("dve", lambda e, pr=pr, tt=tt, hp=hp: e.tensor_mul(
                                    out=kw[pr, 0:128].rearrange("p (h d) -> p h d", h=2),
                                    in0=k_tm[pr, tt, 128 * hp:128 * hp + 128].rearrange("p (h d) -> p h d", h=2),
                                    in1=es_tm[pr, tt, 2 * hp:2 * hp + 2].unsqueeze(2).to_broadcast([64, 2, 64])),
                                    reads=["k_tm", "es_tm"], writes=["kw"])
                                for hh_ in range(2):
                                    h = 2 * hp + hh_
                                    mm(pb[0][64 * hh_:64 * hh_ + 64, 0:129], kw[pr, 64 * hh_:64 * hh_ + 64], v_ml[pr, tt, h, :], True, True,
                                       ["kw", "v_ml"], ["pb0"], fence=True)
                                S.op("dve", lambda e, hp=hp, cg=cg: e.scalar_tensor_tensor(
                                    out=Cst[:, hp, :], in0=Cst[:, hp, :], scalar=decay_bc[:, hp, cg:cg + 1], in1=pb[0][:, 0:129],
                                    op0=ALU.mult, op1=ALU.add),
                                    reads=["Cst", "decay_bc", "pb0"], writes=["Cst"])
                            Hv = pb[0][:, 192:450].rearrange("p (h c) -> p h c", h=2)
                            S.op("act", lambda e, Hv=Hv: e.activation(out=mls[:, 0:2], in_=Hv[:, :, 128], func=AF.Abs),
                                 reads=["pb0"], writes=["mls"])
                            S.op("dve", lambda e, tt=tt, hp=hp: e.tensor_max(out=mls[:, 0:2], in0=mls[:, 0:2], in1=E3_tm[:, tt, 2 * hp:2 * hp + 2]),
                                 reads=["mls", "E3_tm"], writes=["mls"])
                            S.op("dve", lambda e: e.reciprocal(out=mls[:, 0:2], in_=mls[:, 0:2]), reads=["mls"], writes=["mls"])
                            S.op("dve", lambda e, Hv=Hv, hp=hp: e.tensor_mul(out=hh[:, 2 * hp:2 * hp + 2, :], in0=Hv[:, :, 0:128],
                                                                             in1=mls[:, 0:2].unsqueeze(2).to_broadcast([128, 2, 128])),
                                 reads=["pb0", "mls"], writes=["hh"])
                        for h in range(4):
                            S.op("act", lambda e, h=h: e.activation(out=junk[:, 0:128], in_=hh[:, h, :], func=AF.Square, accum_out=mls[:, 4 + h:5 + h]),
                                 reads=["hh"], writes=["junk", "mls"])
                        S.op("act", lambda e: e.activation(out=mls[:, 4:8], in_=mls[:, 4:8], func=AF.Ln, scale=1.0 / 128, bias=1e-6),
                             reads=["mls"], writes=["mls"])
                        S.op("act", lambda e: e.activation(out=mls[:, 4:8], in_=mls[:, 4:8], func=AF.Exp, scale=-0.5), reads=["mls"], writes=["mls"])
                        S.op("dve", lambda e: e.tensor_mul(out=hh[:], in0=hh[:], in1=mls[:, 4:8].unsqueeze(2).to_broadcast([128, 4, 128])),
                             reads=["hh", "mls"], writes=["hh"])
                        S.op("pool", lambda e, tt=tt, ysl=ysl: e.tensor_mul(out=ycat[ysl][:, 512:1024], in0=hh[:].rearrange("p h d -> p (h d)"), in1=og_n[:, tt, :]),
                             reads=["hh", "og_n"], writes=["ycat%d" % ysl])
                        S.op("sp", lambda e, n=n, ysl=ysl: e.dma_start(out=ycat_d[n * 128:(n + 1) * 128, :], in_=ycat[ysl][:]),
                             reads=["ycat%d" % ysl], writes=[("ycat_d", n)], dma="yo%d" % ysl)
                        if debug:
                            S.op("sp", lambda e, n=n, ysl=ysl: e.dma_start(out=dbg["ycat"][n * 128:(n + 1) * 128, :], in_=ycat[ysl][:]),
                                 reads=["ycat%d" % ysl], dma="dyo%d" % ysl)

        def load_w(dst, src_d, nchunk, key, sem):
            for c in range(nchunk):
                S.op("pool", lambda e, c=c: e.dma_start(out=dst[:, c, :], in_=src_d[c * 128:(c + 1) * 128, :]),
                     writes=[(key, c)], dma=sem)
            return [(key, c) for c in range(nchunk)]

        def load_bc(dst, src_d, key, sem):
            S.op("sp", lambda e: e.dma_start(out=dst[:], in_=src_d.partition_broadcast(128)), writes=[key], dma=sem)

        def postnorm(banks, g_bc, tmp, tag, tmpkey):
            s2 = small[tag]
            for hf in range(2):
                S.op("act", lambda e, hf=hf: e.activation(out=tmp[:, hf * 512:(hf + 1) * 512], in_=pb[banks[hf]][:, :], func=AF.Square,
                                                          accum_out=s2[:, hf:hf + 1]),
                     reads=["pb%d" % banks[hf]], writes=[tmpkey, tag])
            S.op("dve", lambda e: e.tensor_add(out=s2[:, 2:3], in0=s2[:, 0:1], in1=s2[:, 1:2]), reads=[tag], writes=[tag])
            S.op("act", lambda e: e.activation(out=s2[:, 2:3], in_=s2[:, 2:3], func=AF.Ln, scale=1.0 / D, bias=1e-6), reads=[tag], writes=[tag])
            S.op("act", lambda e: e.activation(out=s2[:, 2:3], in_=s2[:, 2:3], func=AF.Exp, scale=-0.5), reads=[tag], writes=[tag])
            for hf in range(2):
                S.op("dve", lambda e, hf=hf: e.scalar_tensor_tensor(out=tmp[:, hf * 512:(hf + 1) * 512], in0=pb[banks[hf]][:, :], scalar=s2[:, 2:3],
                                                                    in1=g_bc[:, hf * 512:(hf + 1) * 512], op0=ALU.mult, op1=ALU.mult),
                     reads=["pb%d" % banks[hf], tag, tag + "_g"], writes=[tmpkey])

        if "B1" in phases:
            S.barrier()
            with contextlib.ExitStack() as pbx:
                Bq = lambda name, shape, dt: sbuf(pbx, name, shape, dt)
                w_out = Bq("w_out", [128, 8, D], BF16)
                w_xq = Bq("w_xq", [128, 8, D], BF16)
                w_xo = Bq("w_xo", [128, 8, D], BF16)
                gpost = Bq("gpost", [128, D], F32)
                gxpre = Bq("gxpre", [128, D], F32)
                gxpost = Bq("gxpost", [128, D], F32)
                ones_b = Bq("ones_b", [128, 128], BF16)
                kxT = Bq("kxT", [128, 8, 256], BF16)
                vx = Bq("vx", [128, 2, D], BF16)
                xin = [Bq("xin0", [128, D], F32), Bq("xin1", [128, D], F32)]
                xsbs = [Bq("xsb0", [128, D], BF16), Bq("xsb1", [128, D], BF16)]
                xsb = xsbs[0]
                for nm in ("pn1", "pn2", "xpre_ss", "mem_ss"):
                    small[nm] = Bq(nm, [128, 4], F32) if nm.startswith("pn") else Bq(nm, [128, 1], F32)
                inner = contextlib.ExitStack()
                Bi = lambda name, shape, dt: sbuf(inner, name, shape, dt)
                w_xkv = Bi("w_xkv", [128, 8, 2 * D], BF16)
                gmem = Bi("gmem", [128, D], F32)
                memT = Bi("memT", [128, 8, 256], BF16)
                kw_out = load_w(w_out, w_out_d, 8, "w_out", "b_wout")
                kw_xkv = load_w(w_xkv, w_xkv_d, 8, "w_xkv", "b_wxkv")
                kw_xq = load_w(w_xq, w_xq_d, 8, "w_xq", "b_wxq")
                kw_xo = load_w(w_xo, w_xo_d, 8, "w_xo", "b_wxo")
                load_bc(gpost, gpost_d, "pn1_g", "b_g1")
                load_bc(gxpre, gxpre_d, "gxpre", "b_g2")
                load_bc(gxpost, gxpost_d, "pn2_g", "b_g3")
                load_bc(gmem, gmem_d, "gmem", "b_g4")
                S.op("sp", lambda e: e.dma_start(out=ones_b[:], in_=cd["ones_b"]), writes=["ones_b"], dma="b_ones")
                for mt in range(2):
                    S.op("sp", lambda e, mt=mt: e.dma_start(out=xin[mt][:], in_=mem_d[mt * 128:(mt + 1) * 128, :]), writes=["xin%d" % mt], dma="b_x%d" % mt)
                    rms_prep(xin[mt][:], gmem[:], xsb[:], "mem", "xin%d" % mt, reads_extra=["gmem"], xskey="xsb0")
                    transpose8(xsb[:], memT[:, :, mt * 128:(mt + 1) * 128], "xsb0", "memT")
                for fc in range(8):
                    bi = 1 + fc % 2
                    for k in range(8):
                        mm(pb[bi][:, 0:256], w_xkv[:, k, fc * 128:(fc + 1) * 128], memT[:, k, :], k == 0, k == 7, kw_xkv + ["memT"], ["pb%d" % bi])
                    evac(alt(), kxT[:, fc, :], pb[bi][:, 0:256], ["pb%d" % bi], ["kxT"])
                for mc in range(2):
                    for hf in range(2):
                        bi = 1 + hf
                        for k in range(8):
                            mm(pb[bi][:, :], memT[:, k, mc * 128:(mc + 1) * 128], w_xkv[:, k, D + hf * 512:D + (hf + 1) * 512], k == 0, k == 7,
                               kw_xkv + ["memT"], ["pb%d" % bi])
                        evac(alt(), vx[:, mc, hf * 512:(hf + 1) * 512], pb[bi][:, :], ["pb%d" % bi], ["vx"])

                inner.close()
                S.barrier()
                yb = [Bq("yb0", [128, D], BF16), Bq("yb1", [128, D], BF16)]
                yTs = [Bq("yT0", [128, 8, 128], BF16), Bq("yT1", [128, 8, 128], BF16)]
                x1b = [Bq("x1a", [128, 4, D], F32), Bq("x1b", [128, 4, D], F32)]
                h2T = Bq("h2T", [128, 8, 512], BF16)
                qxT = Bq("qxT", [128, 8, 512], BF16)
                Ex = [Bq("Ex0", [128, 2, 512], BF16), Bq("Ex1", [128, 2, 512], BF16)]
                rZbs = [Bq("rZb0", [128, 512], F32), Bq("rZb1", [128, 512], F32)]
                oxT = Bq("oxT", [128, 8, 512], BF16)
                tmpbs = [Bq("tmpb0", [128, D], F32), Bq("tmpb1", [128, D], F32), Bq("tmpb2", [128, D], F32)]
                x2o = [Bq("x2o0", [128, D], F32), Bq("x2o1", [128, D], F32)]
                bpair = [0]

                def next_pair():
                    bpair[0] ^= 1
                    return (1, 2) if bpair[0] else (3, 4)

                for gi in range(NST):
                    x1 = x1b[gi % 2]
                    x1k = "x1%d" % (gi % 2)
                    for tt in range(4):
                        n = 4 * gi + tt
                        sl = n % 2
                        tsl = slice(tt * 128, (tt + 1) * 128)
                        S.op("sp", lambda e, n=n, sl=sl: e.dma_start(out=yb[sl][:], in_=ycat_d[n * 128:(n + 1) * 128, :]), writes=["yb%d" % sl], dma="b_y%d" % sl)
                        S.op("sp", lambda e, n=n, sl=sl: e.dma_start(out=xin[sl][:], in_=x_d[n * 128:(n + 1) * 128, :]), writes=["xin%d" % sl], dma="b_x%d" % sl)
                        yT = yTs[sl]
                        ytk = "yT%d" % sl
                        tmpb = tmpbs[sl]
                        tk = "tmpb%d" % sl
                        xsb_ = xsbs[sl]
                        xk = "xsb%d" % sl
                        transpose8(yb[sl][:], yT[:], "yb%d" % sl, ytk)
                        bp = next_pair()
                        for hf in range(2):
                            for c in range(8):
                                mm(pb[bp[hf]][:, :], yT[:, c, :], w_out[:, c, hf * 512:(hf + 1) * 512], c == 0, c == 7, kw_out + [ytk], ["pb%d" % bp[hf]])
                        postnorm(bp, gpost, tmpb, "pn1", tk)
                        S.op("pool", lambda e, tt=tt, sl=sl, x1=x1, tmpb=tmpb: e.tensor_add(out=x1[:, tt, :], in0=xin[sl][:], in1=tmpb[:]),
                             reads=["xin%d" % sl, tk], writes=[(x1k, tt)])
                        rms_prep(x1[:, tt, :], gxpre[:], xsb_[:], "xpre", (x1k, tt), reads_extra=["gxpre"], xskey=xk)
                        transpose8(xsb_[:], h2T[:, :, tsl], xk, "h2T", bank=7)
                    for fc in range(8):
                        bi = 1 + fc % 4
                        for k in range(8):
                            mm(pb[bi][:, :], w_xq[:, k, fc * 128:(fc + 1) * 128], h2T[:, k, :], k == 0, k == 7, kw_xq + ["h2T"], ["pb%d" % bi])
                        evac(alt(), qxT[:, fc, :], pb[bi][:, :], ["pb%d" % bi], [("qxT", fc // 2)])
                    for hx in range(4):
                        E = Ex[hx % 2]
                        ek = "Ex%d" % (hx % 2)
                        rZb = rZbs[hx % 2]
                        rk = "rZb%d" % (hx % 2)
                        sb2, zb = ((5, 6), 7) if hx % 2 == 0 else ((1, 2), 3)
                        for mc in range(2):
                            bi = sb2[mc]
                            for dc in range(2):
                                mm(pb[bi][:, :], kxT[:, 2 * hx + dc, mc * 128:(mc + 1) * 128], qxT[:, 2 * hx + dc, :], dc == 0, dc == 1,
                                   ["kxT", ("qxT", hx)], ["pb%d" % bi])
                            S.op("act", lambda e, bi=bi, E=E, mc=mc: e.activation(out=E[:, mc, :], in_=pb[bi][:, :], func=AF.Exp, scale=1.0 / 16),
                                 reads=["pb%d" % bi], writes=[ek])
                        for mc in range(2):
                            mm(pb[zb][:, :], ones_b[:], E[:, mc, :], mc == 0, mc == 1, ["ones_b", ek], ["pb%d" % zb])
                        S.op("act", lambda e, zb=zb, rZb=rZb: e.activation(out=rZb[:], in_=pb[zb][:, :], func=AF.Ln), reads=["pb%d" % zb], writes=[rk])
                        S.op("act", lambda e, rZb=rZb: e.activation(out=rZb[:], in_=rZb[:], func=AF.Exp, scale=-1.0), reads=[rk], writes=[rk])
                        for dvc in range(2):
                            bi = sb2[dvc]
                            for mc in range(2):
                                c0_ = hx * 256 + dvc * 128
                                mm(pb[bi][:, :], vx[:, mc, c0_:c0_ + 128], E[:, mc, :], mc == 0, mc == 1, ["vx", ek], ["pb%d" % bi])
                            S.op("dve", lambda e, bi=bi, hx=hx, dvc=dvc, rZb=rZb: e.tensor_mul(out=oxT[:, 2 * hx + dvc, :], in0=pb[bi][:, :], in1=rZb[:]),
                                 reads=["pb%d" % bi, rk], writes=["oxT"])
                    for tt in range(4):
                        n = 4 * gi + tt
                        sl = n % 2
                        tsl = slice(tt * 128, (tt + 1) * 128)
                        bp = next_pair()
                        for hf in range(2):
                            for c in range(8):
                                mm(pb[bp[hf]][:, :], oxT[:, c, tsl], w_xo[:, c, hf * 512:(hf + 1) * 512], c == 0, c == 7, kw_xo + ["oxT"], ["pb%d" % bp[hf]])
                        postnorm(bp, gxpost, tmpbs[2], "pn2", "tmpb2")
                        S.op("dve", lambda e, tt=tt, sl=sl, x1=x1: e.tensor_add(out=x2o[sl][:], in0=x1[:, tt, :], in1=tmpbs[2][:]),
                             reads=[(x1k, tt), "tmpb2"], writes=["x2o%d" % sl])
                        S.op("sp", lambda e, n=n, sl=sl: e.dma_start(out=x2_d[n * 128:(n + 1) * 128, :], in_=x2o[sl][:]),
                             reads=["x2o%d" % sl], writes=[("x2_d", n)], dma="b_o%d" % sl)
                        if debug:
                            S.op("sp", lambda e, n=n, sl=sl: e.dma_start(out=dbg["x2"][n * 128:(n + 1) * 128, :], in_=x2o[sl][:]),
                                 reads=["x2o%d" % sl], dma="b_do%d" % sl)

        if "B2" in phases:
            S.barrier()
            with contextlib.ExitStack() as pcx:
                Cq = lambda name, shape, dt: sbuf(pcx, name, shape, dt)
                w_gu = Cq("w_gu", [128, 8, 5632], BF16)
                w_dn = Cq("w_dn", [128, 22, D], BF16)
                gfpre = Cq("gfpre", [128, D], F32)
                gfpost = Cq("gfpost", [128, D], F32)
                xin = [Cq("cxin0", [128, D], F32), Cq("cxin1", [128, D], F32)]
                xres = [Cq("xres0", [128, D], F32), Cq("xres1", [128, D], F32)]
                xsb = Cq("cxsb", [128, D], BF16)
                h3T = Cq("h3T", [128, 8, 512], BF16)
                actT = Cq("actT", [128, 22, 512], BF16)
                sgb = [Cq("sgb0", [128, 512], F32), Cq("sgb1", [128, 512], F32)]
                tmpc = Cq("tmpc", [128, D], F32)
                small["pn3"] = Cq("pn3", [128, 4], F32)
                small["fpre_ss"] = Cq("fpre_ss", [128, 1], F32)
                kw_gu = load_w(w_gu, w_gu_d, 8, "w_gu", "c_wgu")
                kgu = {i_: kw_gu for i_ in range(11)}
                kw_dn = load_w(w_dn, w_dn_d, 22, "w_dn", "c_wdn")
                load_bc(gfpre, gfpre_d, "gfpre", "c_g1")
                load_bc(gfpost, gfpost_d, "pn3_g", "c_g2")
                for gi in range(NST):
                    for tt in range(4):
                        n = 4 * gi + tt
                        sl = n % 2
                        S.op("sp", lambda e, n=n, sl=sl: e.dma_start(out=xin[sl][:], in_=x2_d[n * 128:(n + 1) * 128, :]),
                             reads=[("x2_d", n)], writes=["cxin%d" % sl], dma="c_x%d" % sl)
                        rms_prep(xin[sl][:], gfpre[:], xsb[:], "fpre", "cxin%d" % sl, reads_extra=["gfpre"])
                        transpose8(xsb[:], h3T[:, :, tt * 128:(tt + 1) * 128], "fpre_xs", "h3T")
                    for fc in range(22):
                        bg, bu = (1, 2) if fc % 2 == 0 else (3, 4)
                        for k in range(8):
                            mm(pb[bg][:, :], w_gu[:, k, fc * 128:(fc + 1) * 128], h3T[:, k, :], k == 0, k == 7, kgu[fc // 2] + ["h3T"], ["pb%d" % bg])
                        for k in range(8):
                            mm(pb[bu][:, :], w_gu[:, k, 2816 + fc * 128:2816 + (fc + 1) * 128], h3T[:, k, :], k == 0, k == 7, kgu[fc // 2] + ["h3T"], ["pb%d" % bu])
                        sg = sgb[fc % 2]
                        S.op("act", lambda e, sg=sg, bg=bg: e.activation(out=sg[:], in_=pb[bg][:, :], func=AF.Silu), reads=["pb%d" % bg], writes=["sgb%d" % (fc % 2)])
                        S.op("dve", lambda e, sg=sg, bu=bu, fc=fc: e.tensor_mul(out=actT[:, fc, :], in0=pb[bu][:, :], in1=sg[:]),
                             reads=["pb%d" % bu, "sgb%d" % (fc % 2)], writes=[("actT", fc)])
                    for tt in range(4):
                        n = 4 * gi + tt
                        sl = n % 2
                        tsl = slice(tt * 128, (tt + 1) * 128)
                        S.op("sp", lambda e, n=n, sl=sl: e.dma_start(out=xres[sl][:], in_=x2_d[n * 128:(n + 1) * 128, :]),
                             reads=[("x2_d", n)], writes=["xres%d" % sl], dma="c_r%d" % sl)
                        bp = (5, 6)
                        for hf in range(2):
                            for fc in range(22):
                                mm(pb[bp[hf]][:, :], actT[:, fc, tsl], w_dn[:, fc, hf * 512:(hf + 1) * 512], fc == 0, fc == 21,
                                   kw_dn + [("actT", fc)], ["pb%d" % bp[hf]])
                        postnorm(bp, gfpost, tmpc, "pn3", "tmpc")
                        S.op("pool", lambda e, sl=sl: e.tensor_add(out=xres[sl][:], in0=xres[sl][:], in1=tmpc[:]),
                             reads=["xres%d" % sl, "tmpc"], writes=["xres%d" % sl])
                        S.op("sp", lambda e, n=n, sl=sl: e.dma_start(out=out_d[n * 128:(n + 1) * 128, :], in_=xres[sl][:]),
                             reads=["xres%d" % sl], dma="c_o%d" % sl)

        S.emit(reorder=REORDER)
    return nc


def w_in_perm():
    cols = []
    for g in range(4):
        cols += list(range(64 * g, 64 * g + 64)) + list(range(64 * (4 + g), 64 * (4 + g) + 64))
    for b0 in (512, 576, 640, 704):
        cols += list(range(b0, b0 + 64)) * 2
    cols += list(range(768, 896)) + list(range(1024, 1152))
    cols += list(range(1304, 1560)) + list(range(1560, 1816))
    cols += list(range(2328, 2332)) + list(range(2332, 2336))
    cols += list(range(896, 1024)) + list(range(1152, 1280)) + list(range(1280, 1304))
    cols += list(range(1816, 2328))
    cols += list(range(2336, 2848))
    assert len(cols) == 3104 and len(set(cols)) == 2848
    return np.array(cols)


def shared_inputs(inp):
    f = lambda a: np.ascontiguousarray(np.asarray(a, dtype=np.float32))
    m = {}
    rb = np.zeros((33, 8), np.float32)
    rb[0:32] = f(inp["rel_bias"]).T
    rb[32] = NEG
    m["rel_biasT"] = rb
    m["w_in_p"] = f(f(inp["w_in"])[0][:, w_in_perm()])
    m["mix_norm_pre"] = f(inp["mix_norm_pre"])
    for nm in ("cmp_pos_k", "cmp_pos_v"):
        p = f(inp[nm])[0]
        m[nm] = f(p.reshape(16, 2, 64).transpose(1, 2, 0).reshape(128, 16))
    for nm in ("cmp_w1_k", "cmp_w1_v", "cmp_w2_k", "cmp_w2_v", "w_out", "w_xq", "w_xkv", "w_xo", "w_gate_up", "w_down"):
        m[nm] = f(inp[nm])[0]
    cw = f(inp["conv_w"])[0]
    m["conv_w_p"] = f(cw.reshape(4, 4, 128).transpose(2, 1, 0))
    m["conv_b_p"] = f(f(inp["conv_b"])[0].reshape(4, 128).T)
    m["gate_bias_p"] = f(f(inp["mlstm_gate_bias"])[0].T)
    for nm in ("mlstm_norm", "mix_norm_post", "xattn_norm_pre", "mem_norm", "xattn_norm_post", "ffn_norm_pre", "ffn_norm_post"):
        m[nm] = f(inp[nm])
    c = host_consts()
    for n_ in CONST_NAMES:
        m["c_" + n_] = c[n_]
    return m


_NC_CACHE = {}


def kernel(**inputs):
    x = np.asarray(inputs["x"], dtype=np.float32)
    mem = np.asarray(inputs["mem"], dtype=np.float32)
    B, S_TOK, _ = x.shape
    if S_TOK not in _NC_CACHE:
        _NC_CACHE[S_TOK] = build_program(S_TOK)
    nc = _NC_CACHE[S_TOK]
    shared = shared_inputs(inputs)
    in_maps = []
    for b in range(B):
        m = dict(shared)
        m["x"] = np.ascontiguousarray(x[b])
        m["mem"] = np.ascontiguousarray(mem[b])
        in_maps.append(m)
    res = run_bass_kernel_spmd(nc, in_maps, core_ids=list(range(B)))
    return np.stack([np.asarray(r["out"], dtype=np.float32) for r in res.results], axis=0)
```

```python
import contextlib
import numpy as np
import ml_dtypes
import concourse.bass as bass
import concourse.mybir as mybir
from concourse.bass_utils import run_bass_kernel_spmd

F32 = mybir.dt.float32
BF16 = mybir.dt.bfloat16
AF = mybir.ActivationFunctionType
ALU = mybir.AluOpType
AX = mybir.AxisListType
NPBF = ml_dtypes.bfloat16

D = 1024
NEG = -30000.0
ENGS = ("pe", "act", "dve", "pool", "sp")
REORDER = True


class _Rec:
    def __init__(self):
        self.call = None

    def __getattr__(self, name):
        def f(*a, **kw):
            self.call = (name, a, kw)
            return None
        return f


class Sched:
    def __init__(self, nc):
        self.nc = nc
        self.ops = []
        self.last_writer = {}
        self.readers = {}
        self.bounds = []

    def barrier(self):
        self.bounds.append(len(self.ops))

    @staticmethod
    def _cost(eng, meth, a, kw):
        def fsz(ap):
            n = 1
            for d in ap.shape[1:]:
                n *= int(d)
            return n
        try:
            if meth == "matmul":
                n = fsz(kw["rhs"])
                t = max(0.07, n * 0.0006)
                if kw["rhs"].dtype == F32:
                    t *= 4
                return t, t + 0.1
            if meth == "transpose" and eng == "pe":
                return 0.12, 0.3
            if meth == "dma_start":
                ap = kw["out"]
                n = fsz(ap) * int(ap.shape[0])
                return 0.15, 3.0 + n * 4 / 150e3
            ap = kw.get("out", kw.get("ap", a[0] if a else None))
            n = fsz(ap)
            if eng == "act":
                t = 0.22 + n * 0.00078
            elif eng == "dve":
                t = 0.15 + n * 0.00105
            else:
                t = 0.3 + n * 0.0034
            return t, t + 0.15
        except Exception:
            return 0.3, 0.5

    def _schedule_segment(self, lo, hi):
        import heapq
        ops = self.ops
        ndeps = {}
        users = {}
        for i in range(lo, hi):
            ds = [d for d in ops[i]["deps"] if d >= lo]
            ndeps[i] = len(ds)
            for d in ds:
                users.setdefault(d, []).append(i)
        tfree = {e: 0.0 for e in ENGS}
        Ht = {e: [] for e in ENGS}
        Hi = {e: [] for e in ENGS}
        end = {}
        ready = {}
        for i in range(lo, hi):
            if ndeps[i] == 0:
                ready[i] = 0.0
                heapq.heappush(Ht[ops[i]["eng"]], (0.0, i))
        order = []
        nleft = hi - lo
        pemode = [None]
        SWITCH = 0.25
        while nleft:
            best = None
            for e in ENGS:
                while Ht[e] and Ht[e][0][0] <= tfree[e]:
                    _, i = heapq.heappop(Ht[e])
                    heapq.heappush(Hi[e], i)
                if e == "pe" and Hi[e] and ops[Hi[e][0]]["mode"] != pemode[0]:
                    same = [j for j in Hi[e][:24] if ops[j]["mode"] == pemode[0]]
                    if same:
                        j = min(same)
                        Hi[e].remove(j)
                        heapq.heapify(Hi[e])
                        cand = (tfree[e], j, e, None)
                    else:
                        cand = (tfree[e] + SWITCH, Hi[e][0], e, True)
                elif Hi[e]:
                    cand = (tfree[e], Hi[e][0], e, True)
                elif Ht[e]:
                    cand = (Ht[e][0][0], Ht[e][0][1], e, False)
                else:
                    continue
                if cand[3] is None:
                    pass
                if best is None or cand[:2] < best[:2]:
                    if best is not None and best[3] is None:
                        heapq.heappush(Hi[best[2]], best[1])
                    best = cand
                elif cand[3] is None:
                    heapq.heappush(Hi[e], cand[1])
            st, i, e, fromidx = best
            if fromidx is None:
                pass
            elif fromidx:
                heapq.heappop(Hi[e])
            else:
                heapq.heappop(Ht[e])
            if e == "pe":
                pemode[0] = ops[i]["mode"]
            busy, lat = ops[i]["cost"]
            tfree[e] = st + busy
            end[i] = st + lat
            order.append(i)
            nleft -= 1
            for u in users.get(i, ()):
                ndeps[u] -= 1
                ready[u] = max(ready.get(u, 0.0), end[i])
                if ndeps[u] == 0:
                    heapq.heappush(Ht[ops[u]["eng"]], (ready[u], u))
        self.makespan = getattr(self, "makespan", 0.0) + max(end.values())
        return order

    def finalize(self, reorder=True):
        ops = self.ops
        bounds = [0] + self.bounds + [len(ops)]
        order = []
        for lo, hi in zip(bounds[:-1], bounds[1:]):
            if hi > lo:
                order += self._schedule_segment(lo, hi) if reorder else list(range(lo, hi))
        newidx = {old: new for new, old in enumerate(order)}
        seg_of = {}
        for si, (lo, hi) in enumerate(zip(bounds[:-1], bounds[1:])):
            for i in range(lo, hi):
                seg_of[i] = si
        newops = []
        for old in order:
            o = ops[old]
            o["deps"] = sorted(newidx[d] for d in o["deps"])
            o["seg"] = seg_of[old]
            newops.append(o)
        last = {}
        seen = set()
        cur = 0
        for i, o in enumerate(newops):
            if o["seg"] != cur:
                cur = o["seg"]
                seen = set()
                self._bar = set(last.values())
            if cur > 0 and o["eng"] not in seen:
                seen.add(o["eng"])
                o["deps"] = sorted(set(o["deps"]) | self._bar)
            if o["dma"] is not None:
                last[("dma", o["dma"])] = i
            else:
                last[("eng", o["eng"])] = i
        self.ops = newops

    def op(self, eng, fn, reads=(), writes=(), dma=None, fence=False):
        i = len(self.ops)
        writes = list(writes) + [k for k in reads if isinstance(k, str) and k.startswith("pb")]
        reads = [k for k in reads if not (isinstance(k, str) and k.startswith("pb"))]
        deps = set()
        for k in list(reads) + list(writes):
            w = self.last_writer.get(k)
            if w is not None:
                deps.add(w)
        for k in writes:
            for r in self.readers.get(k, ()):
                deps.add(r)
        deps.discard(i)
        for k in writes:
            self.last_writer[k] = i
            self.readers[k] = []
        for k in reads:
            self.readers.setdefault(k, []).append(i)
        rec = _Rec()
        fn(rec)
        assert rec.call is not None
        meth, a, kw = rec.call
        mode = None
        if eng == "pe":
            try:
                l_ = kw.get("lhsT", kw.get("in_"))
                mode = "full" if (int(l_.shape[0]) == 128 and l_.dtype != F32) else "tile"
            except Exception:
                mode = "tile"
        self.ops.append(dict(eng=eng, fn=(lambda e: getattr(e, meth)(*a, **kw)), deps=sorted(deps), dma=dma, fence=fence,
                             cost=self._cost(eng, meth, a, kw), mode=mode))
        return i

    def emit(self, reorder=True):
        self.finalize(reorder)
        nc = self.nc
        ops = self.ops
        needed = [False] * len(ops)
        for o in ops:
            for d in o["deps"]:
                od = ops[d]
                if od["dma"] is None and od["eng"] == "pe" and o["eng"] == "pe" and o["dma"] is None and not o["fence"]:
                    continue
                needed[d] = True
        eng_cnt = {e: 0 for e in ENGS}
        dma_cnt = {}
        for i, o in enumerate(ops):
            if o["dma"] is not None:
                k = o["dma"]
                dma_cnt[k] = dma_cnt.get(k, 0) + 16
                o["sem"] = ("dma", k)
                o["val"] = dma_cnt[k]
            else:
                e = o["eng"]
                if needed[i]:
                    eng_cnt[e] += 1
                    o["sem"] = ("eng", e)
                    o["val"] = eng_cnt[e]
                else:
                    o["sem"] = None
        sem_names = [("eng", e) for e in ENGS if e != "sp"] + [("dma", k) for k in dma_cnt]
        with contextlib.ExitStack() as st:
            sems = {}
            for sn in sem_names:
                sems[sn] = st.enter_context(nc.semaphore("s_%s_%s" % sn))
            block = st.enter_context(nc.Block())
            per_eng = {e: [] for e in ENGS}
            for i, o in enumerate(ops):
                per_eng[o["eng"]].append(i)
            final_dma = dict(dma_cnt)

            def make_body(e):
                def body(eng):
                    waited = {}
                    for i in per_eng[e]:
                        o = ops[i]
                        need = {}
                        for d in o["deps"]:
                            od = ops[d]
                            if od["sem"] is None:
                                continue
                            if od["sem"] == ("eng", "pe") and e == "pe" and o["dma"] is None and not o["fence"]:
                                continue
                            sn, v = od["sem"], od["val"]
                            if v > need.get(sn, 0):
                                need[sn] = v
                        for sn, v in need.items():
                            if waited.get(sn, 0) >= v:
                                continue
                            waited[sn] = v
                            eng.wait_ge(sems[sn], v)
                        ins = o["fn"](eng)
                        if o["sem"] is not None:
                            ins.then_inc(sems[o["sem"]], 16 if o["dma"] is not None else 1)
                    if e == "sp":
                        for k, v in final_dma.items():
                            eng.wait_ge(sems[("dma", k)], v)
                return body

            block.tensor(make_body("pe"))
            block.scalar(make_body("act"))
            block.vector(make_body("dve"))
            block.gpsimd(make_body("pool"))
            block.sync(make_body("sp"))


def rel_bucket_np(dist):
    n = np.maximum(dist, 0)
    nf = np.maximum(n, 1).astype(np.float32)
    large = 16 + (np.log(nf / np.float32(16)) / np.float32(np.log(128 / 16)) * np.float32(16)).astype(np.int32)
    large = np.minimum(large, 31)
    return np.where(n < 16, n, large)


def host_consts():
    c = {}
    c["ident_f"] = np.eye(128, dtype=np.float32)
    c["ident_b"] = np.eye(128, dtype=np.float32).astype(NPBF)
    c["anti_b"] = np.eye(128, dtype=np.float32)[::-1].copy().astype(NPBF)
    c["ones_b"] = np.ones((128, 128), np.float32).astype(NPBF)
    idx = np.arange(384)
    dist = idx - 127
    oh = np.zeros((33, 384), np.float32)
    b = rel_bucket_np(dist)
    for i in range(384):
        if dist[i] >= 0:
            oh[b[i], i] = 1.0
            oh[31, i] -= 1.0
        else:
            oh[32, i] = 1.0
    c["onehot"] = oh
    kl = np.arange(128)[:, None]
    tl = np.arange(128)[None, :]
    c["d4"] = np.where(kl > tl, 0.0, NEG).astype(np.float32).astype(NPBF)
    sw = np.zeros((17, 384), np.float32)
    for k in range(16):
        sw[k, 254 - k] = 1.0
    sw[16, 255:] = 1.0
    c["shiftw"] = sw.astype(NPBF)
    tlv = np.arange(128)
    c0 = (tlv >= 64).astype(np.int64)
    k = np.arange(126)[None, :] - 62
    forced = (k == c0[:, None]) | (k == c0[:, None] - 1)
    valid = k <= c0[:, None]
    vm = (valid & ~forced).astype(np.float32)
    add = np.where(forced, 1.0e4, np.where(valid, 0.0, -1.0)).astype(np.float32)
    c["vm_tab"] = vm
    c["add_tab"] = add
    va = np.zeros((128, 2, 2, 129), np.float32)
    for ch in range(2):
        for p in range(128):
            cc = ch * 128 + p
            if cc >= 255:
                continue
            va[p, :, ch, 64] = 1.0
            for j in range(64):
                if 16 * cc <= 64 * j + 63 and 16 * cc + 31 >= 64 * j:
                    va[p, :, ch, 65 + j] = 1.0
    c["vcmp_init"] = va.astype(NPBF)
    s = np.arange(128)[:, None] % 64
    t = np.arange(64)[None, :]
    c["tri_ml"] = (s <= t).astype(np.float32)
    rm = np.ones((4, 512), np.float32)
    rm[:, 0::64] = 0.0
    c["resetmask"] = rm
    nr = np.zeros((4, 512), np.float32)
    nr[:, 0::64] = -1.0e30
    c["negreset"] = nr
    selbc = np.zeros((4, 4, 64), np.float32)
    for h in range(4):
        selbc[h, h, :] = 1.0
    c["selbc"] = selbc.astype(NPBF)
    selpair = np.zeros((4, 2, 128), np.float32)
    for cp in range(2):
        selpair[2 * cp, cp, 0:64] = 1.0
        selpair[2 * cp + 1, cp, 64:128] = 1.0
    c["selpair"] = selpair
    ex = np.zeros((64, 4096), np.float32)
    for j in range(64):
        ex[j, 64 * j:64 * j + 64] = 1.0
    c["expand"] = ex.astype(NPBF)
    return c


CONST_NAMES = ["ident_f", "ident_b", "anti_b", "ones_b", "onehot", "d4", "shiftw", "vm_tab", "add_tab",
               "vcmp_init", "tri_ml", "resetmask", "negreset", "selbc", "selpair", "expand"]


def np_dt(a):
    return BF16 if a.dtype == NPBF else F32


def build_program(S_TOK=4096, phases=("A", "B1", "B2"), debug=False):
    NT = S_TOK // 128
    NST = S_TOK // 512
    nc = bass.Bass("TRN2", target_bir_lowering=False)
    consts_np = host_consts()

    def din(name, shape, dt=F32):
        return nc.dram_tensor(name, list(shape), dt, kind="ExternalInput").ap()

    x_d = din("x", [S_TOK, D])
    mem_d = din("mem", [256, D])
    relb_d = din("rel_biasT", [33, 8])
    w_in_d = din("w_in_p", [D, 3104])
    gpre_d = din("mix_norm_pre", [1, D])
    posk_d = din("cmp_pos_k", [128, 16])
    posv_d = din("cmp_pos_v", [128, 16])
    w1k_d = din("cmp_w1_k", [2048, 256])
    w1v_d = din("cmp_w1_v", [2048, 256])
    w2k_d = din("cmp_w2_k", [256, 64])
    w2v_d = din("cmp_w2_v", [256, 64])
    convw_d = din("conv_w_p", [128, 4, 4])
    convb_d = din("conv_b_p", [128, 4])
    gb_d = din("gate_bias_p", [4, 2])
    mln_d = din("mlstm_norm", [1, 512])
    w_out_d = din("w_out", [D, D])
    gpost_d = din("mix_norm_post", [1, D])
    gxpre_d = din("xattn_norm_pre", [1, D])
    gmem_d = din("mem_norm", [1, D])
    w_xq_d = din("w_xq", [D, D])
    w_xkv_d = din("w_xkv", [D, 2 * D])
    w_xo_d = din("w_xo", [D, D])
    gxpost_d = din("xattn_norm_post", [1, D])
    gfpre_d = din("ffn_norm_pre", [1, D])
    w_gu_d = din("w_gate_up", [D, 5632])
    w_dn_d = din("w_down", [2816, D])
    gfpost_d = din("ffn_norm_post", [1, D])
    cd = {n: din("c_" + n, consts_np[n].shape, np_dt(consts_np[n])) for n in CONST_NAMES}

    out_d = nc.dram_tensor("out", [S_TOK, D], F32, kind="ExternalOutput").ap()
    ycat_d = nc.dram_tensor("ycat_scr", [S_TOK, D], BF16, kind="Internal").ap()
    x2_d = nc.dram_tensor("x2_scr", [S_TOK, D], F32, kind="Internal").ap()
    F_d = nc.dram_tensor("F_scr", [8, 384], F32, kind="Internal").ap()
    dbg = {}
    if debug:
        dbg["ycat"] = nc.dram_tensor("dbg_ycat", [S_TOK, D], BF16, kind="ExternalOutput").ap()
        dbg["x2"] = nc.dram_tensor("dbg_x2", [S_TOK, D], F32, kind="ExternalOutput").ap()

    S = Sched(nc)
    rr = [0]

    def alt():
        rr[0] ^= 1
        return "act" if rr[0] else "dve"

    def evac(eng, out, in_, reads, writes, scale=None):
        if eng == "act":
            if scale is None:
                S.op("act", lambda e: e.copy(out=out, in_=in_), reads=reads, writes=writes)
            else:
                S.op("act", lambda e: e.activation(out=out, in_=in_, func=AF.Copy, scale=scale), reads=reads, writes=writes)
        else:
            if scale is None:
                S.op("dve", lambda e: e.tensor_copy(out=out, in_=in_), reads=reads, writes=writes)
            else:
                S.op("dve", lambda e: e.tensor_scalar_mul(out=out, in0=in_, scalar1=scale), reads=reads, writes=writes)

    def mm(out, lhsT, rhs, start, stop, reads, writes, skip=False, fence=False):
        S.op("pe", lambda e: e.matmul(out, lhsT=lhsT, rhs=rhs, start=start, stop=stop, skip_group_check=skip),
             reads=reads, writes=writes, fence=fence)

    with contextlib.ExitStack() as top:
        pb = [top.enter_context(nc.psum_tensor("pb%d" % i, [128, 512], F32)) for i in range(8)]

        def pbf(i):
            return pb[i][:].bitcast(BF16)

        def sbuf(st, name, shape, dt):
            return st.enter_context(nc.sbuf_tensor("sb_" + name, list(shape), dt))

        ident_f = sbuf(top, "ident_f", [128, 128], F32)
        ident_b = sbuf(top, "ident_b", [128, 128], BF16)
        junk = sbuf(top, "junk", [128, 128], BF16)
        S.op("sp", lambda e: e.dma_start(out=ident_f[:], in_=cd["ident_f"]), writes=["ident_f"], dma="c0")
        S.op("sp", lambda e: e.dma_start(out=ident_b[:], in_=cd["ident_b"]), writes=["ident_b"], dma="c1")

        def rms_prep(xt_ap, g_bc, xs_out, tag, xkey, reads_extra=(), xskey=None):
            ss = small[tag + "_ss"]
            xskey = xskey or (tag + "_xs")
            S.op("act", lambda e: e.activation(out=xs_out, in_=xt_ap, func=AF.Square, accum_out=ss[:]),
                 reads=[xkey], writes=[xskey, tag + "_ss"])
            S.op("act", lambda e: e.activation(out=ss[:], in_=ss[:], func=AF.Ln, scale=1.0 / D, bias=1e-6),
                 reads=[tag + "_ss"], writes=[tag + "_ss"])
            S.op("act", lambda e: e.activation(out=ss[:], in_=ss[:], func=AF.Exp, scale=-0.5), reads=[tag + "_ss"], writes=[tag + "_ss"])
            S.op("dve", lambda e: e.scalar_tensor_tensor(out=xs_out, in0=xt_ap, scalar=ss[:], in1=g_bc, op0=ALU.mult,
                                                          op1=ALU.mult),
                 reads=[xkey, tag + "_ss"] + list(reads_extra), writes=[xskey])

        small = {}

        def transpose8(xs_ap, dst_ap, skey, dkey, bank=0):
            pv = pbf(bank)
            bk = "pb%d" % bank
            for c in range(8):
                S.op("pe", lambda e, c=c: e.transpose(out=pv[:, c * 128:(c + 1) * 128], in_=xs_ap[:, c * 128:(c + 1) * 128],
                                                      identity=ident_b[:]),
                     reads=[skey, "ident_b"], writes=[bk])
            evac(alt(), dst_ap, pv.rearrange("p (c t) -> p c t", c=8), reads=[bk], writes=[dkey])

        if "A" in phases:
            with contextlib.ExitStack() as pa:
                A = lambda name, shape, dt: sbuf(pa, name, shape, dt)
                w_in = A("w_in", [128, 8, 3104], BF16)
                gpre = A("gpre", [128, D], F32)
                w1 = [A("w1k", [128, 16, 256], BF16), A("w1v", [128, 16, 256], BF16)]
                w2k = A("w2k", [128, 2, 128], BF16)
                w2v = A("w2v", [128, 2, 64], BF16)
                pos = [A("posk", [128, 16], BF16), A("posv", [128, 16], BF16)]
                c1 = [A("c1k", [128, 2], F32), A("c1v", [128, 2], F32)]
                anti_b = A("anti_b", [128, 128], BF16)
                d4 = A("d4", [128, 128], BF16)
                shiftw = A("shiftw", [17, 384], BF16)
                vm_tab = A("vm_tab", [128, 126], F32)
                add_tab = A("add_tab", [128, 126], F32)
                tri_ml = A("tri_ml", [128, 64], F32)
                resetmask = A("resetmask", [4, 512], F32)
                negreset = A("negreset", [4, 512], F32)
                selbc = A("selbc", [4, 4, 64], BF16)
                gRAb = A("gRAb", [4, 8, 2, 64], BF16)
                selpair = A("selpair", [4, 2, 128], F32)
                relbT = A("relbT", [33, 8], F32)
                G0 = A("G0", [128, 8, 128], BF16)
                G1 = A("G1", [128, 8, 128], BF16)
                bandx = A("bandx", [17, 8, 128], BF16)
                convw = A("convw", [128, 4, 4], F32)
                convb = A("convb", [128, 4], F32)
                gb = A("gb", [4, 2], F32)
                ngb1 = A("ngb1", [4, 1], F32)
                mln = A("mln", [128, 512], F32)
                kTs = [A("kTs0", [128, S_TOK], BF16), A("kTs1", [128, S_TOK], BF16)]
                kT_win = A("kT_win", [128, S_TOK], BF16)
                V_slc = A("V_slc", [128, NT, 2, 65], BF16)
                V_win = A("V_win", [128, NT, 2, 65], BF16)
                k_cmpT = A("k_cmpT", [128, 256], BF16)
                hidT_v = A("hidT_v", [128, 2, 2, 256], BF16)
                hid_k = A("hid_k", [128, 2, 2, 32], BF16)
                vcmp = A("vcmp", [128, 2, 2, 129], BF16)
                xt = [A("xt0", [128, D], F32)] * 2
                xs = A("xs", [128, D], BF16)
                hT = A("hT", [128, 8, 512], BF16)
                qTz = [A("qTz0", [128, 4, 4, 128], BF16), A("qTz1", [128, 4, 4, 128], BF16)]
                kc2 = [[A("kc2_%d%d" % (kv, h), [128, 528], BF16) for h in range(2)] for kv in range(2)]
                uT = A("uT", [128, 4, 515], BF16)
                qkml = A("qkml", [128, 4, 512], BF16)
                k_tm = A("k_tm", [128, 4, 256], BF16)
                v_ml = A("v_ml", [128, 4, 4, 129], BF16)
                og_n = A("og_n", [128, 4, 512], BF16)
                gates = A("gates", [128, 4, 24], F32)
                gI = A("gI", [4, 512], F32)
                gL = A("gL", [4, 512], F32)
                gG = A("gG", [4, 512], F32)
                gRA = A("gRA", [4, 8, 2, 64], F32)
                gE3 = A("gE3", [4, 512], F32)
                gsm = A("gsm", [4, 64], F32)
                E3_tm = A("E3_tm", [128, 4, 4], F32)
                es_tm = A("es_tm", [128, 4, 4], F32)
                decay_bc = A("decay_bc", [128, 2, 8], F32)
                Cst = A("Cst", [128, 2, 129], F32)
                qba = A("qba", [128, 2, 64], F32)
                qbb = A("qbb", [128, 64], BF16)
                STb = A("STb", [128, 64], BF16)
                kw = A("kw", [128, 256], BF16)
                hh = A("hh", [128, 4, 128], F32)
                cacc = hh[:].rearrange("p h d -> p (h d)")
                mls = A("mls", [128, 16], F32)
                Eb = [A("Eb0", [128, 4, 128], BF16), A("Eb1", [128, 4, 128], BF16), A("Eb2", [128, 4, 128], BF16)]
                zrow = A("zrow", [1, 512], BF16)
                asm = A("asm", [128, 32], F32)
                imp = A("imp", [128, 64], F32)
                sc = A("sc", [128, 64], F32)
                sc2 = imp
                m8 = A("m8", [128, 16], F32)
                negsel = A("negsel", [128, 64], BF16)
                qTs = [A("qTs0", [128, 4, 128], BF16), A("qTs1", [128, 4, 128], BF16)]
                cmp_sb = A("cmp_sb", [128, 516], F32)
                ytmp = cmp_sb[:, 0:256].rearrange("p (g d) -> p g d", g=4)
                yacc = A("yacc", [128, 512], F32)
                ycat = [A("ycat0", [128, D], BF16)] * 2
                ycf = ycat[0][:].bitcast(F32)
                onehot = ycf[0:33, 0:384]
                Fsb = ycf[64:72, 0:384]
                for nm in ("pre_ss",):
                    small[nm] = A(nm, [128, 1], F32)

                def ld(dst, src, key, sem, eng="sp"):
                    S.op(eng, lambda e: e.dma_start(out=dst, in_=src), writes=[key], dma=sem)

                for c in range(8):
                    ld(w_in[:, c, :], w_in_d[c * 128:(c + 1) * 128, :], "w_in", "win%d" % c, eng="pool")
                ld(gpre[:], gpre_d.partition_broadcast(128), "gpre", "c2")
                ld(w1[0][:], w1k_d.rearrange("(j p) n -> p j n", p=128), "w1k", "c3", eng="pool")
                ld(w1[1][:], w1v_d.rearrange("(j p) n -> p j n", p=128), "w1v", "c4", eng="pool")
                ld(w2k[:, :, 0:64], w2k_d.rearrange("(c p) n -> p c n", p=128), "w2k", "c5", eng="pool")
                ld(w2k[:, :, 64:128], w2k_d.rearrange("(c p) n -> p c n", p=128), "w2k", "c6", eng="pool")
                ld(w2v[:], w2v_d.rearrange("(c p) n -> p c n", p=128), "w2v", "c7", eng="pool")
                ld(pos[0][:], posk_d, "posk", "c8", eng="pool")
                ld(pos[1][:], posv_d, "posv", "c9", eng="pool")
                for (t_, nm) in ((anti_b, "anti_b"), (d4, "d4"), (shiftw, "shiftw"), (vm_tab, "vm_tab"),
                                 (add_tab, "add_tab"), (tri_ml, "tri_ml"), (resetmask, "resetmask"),
                                 (negreset, "negreset"), (selbc, "selbc"), (selpair, "selpair"),
                                 (vcmp, "vcmp_init")):
                    key = "vcmp" if nm == "vcmp_init" else nm
                    ld(t_[:], cd[nm], key, "k_" + nm)
                ld(relbT[:], relb_d, "relbT", "c10")
                ld(onehot, cd["onehot"], "ycat0", "k_onehot")
                ld(kTs[0][64:128, :], cd["expand"][:, 0:S_TOK], "kTs_e0", "c15")
                ld(kTs[1][0:64, :], cd["expand"][:, 0:S_TOK], "kTs_e1", "c16")
                ld(convw[:], convw_d, "convw", "c11")
                ld(convb[:], convb_d, "convb", "c12")
                ld(gb[:], gb_d, "gb", "c13")
                ld(mln[:], mln_d.partition_broadcast(128), "mln", "c14")

                S.op("pool", lambda e: e.memset(V_slc[:], 1.0), writes=["V_slc"])
                S.op("pool", lambda e: e.memset(V_win[:], 1.0), writes=["V_win"])
                S.op("pool", lambda e: e.memset(v_ml[:], 1.0), writes=["v_ml"])
                S.op("pool", lambda e: e.memset(hidT_v[:], 0.0), writes=["hidT_v"])
                S.op("pool", lambda e: e.memset(k_cmpT[:], 0.0), writes=["k_cmpT"])
                S.op("pool", lambda e: e.memset(uT[:], 0.0), writes=["uT"])
                S.op("pool", lambda e: e.memset(Cst[:], 0.0), writes=["Cst"])
                S.op("pool", lambda e: e.memset(gsm[:], 0.0), writes=["gsm"])
                S.op("pool", lambda e: e.memset(zrow[:], 0.0), writes=["zrow"])
                S.op("pool", lambda e: e.memset(qTz[0][:], 0.0), writes=["qT"])
                S.op("pool", lambda e: e.memset(qTz[1][:], 0.0), writes=["qT"])
                for kv in range(2):
                    for h in range(2):
                        S.op("pool", lambda e, kv=kv, h=h: e.memset(kc2[kv][h][:], 0.0), writes=["kc2_%d%d" % (kv, h)])
                S.op("dve", lambda e: e.tensor_scalar_mul(out=ngb1[:], in0=gb[:, 1:2], scalar1=-1.0), reads=["gb"], writes=["ngb1"])

                hTf = hT[:].rearrange("p a b -> p (a b)").bitcast(F32)
                G0f = hTf[:, 0:1024].rearrange("p (h t) -> p h t", h=8)
                bandf = hTf[0:17, 1024:2048].rearrange("p (h t) -> p h t", h=8)
                mm(pb[1][64:72, 0:384], relbT[:], onehot, True, True, ["relbT", "ycat0"], ["pb1"])
                evac("dve", Fsb, pb[1][64:72, 0:384], ["pb1"], ["ycat0"])
                S.op("sp", lambda e: e.dma_start(out=F_d, in_=Fsb), reads=["ycat0"], writes=["F_d"], dma="fst")
                Ft = F_d.tensor
                S.op("sp", lambda e: e.dma_start(out=G0f, in_=bass.AP(tensor=Ft, offset=0, ap=[[1, 128], [384, 8], [1, 128]])),
                     reads=["F_d"], writes=["hT"], dma="fl0")
                S.op("dve", lambda e: e.tensor_copy(out=G0[:], in_=G0f), reads=["hT"], writes=["G0"])
                S.op("sp", lambda e: e.dma_start(out=G0f, in_=bass.AP(tensor=Ft, offset=128, ap=[[1, 128], [384, 8], [1, 128]])),
                     reads=["F_d"], writes=["hT"], dma="fl1")
                S.op("dve", lambda e: e.tensor_copy(out=G1[:], in_=G0f), reads=["hT"], writes=["G1"])
                S.op("pool", lambda e: e.memset(bandf, NEG), writes=["hT"])
                S.op("sp", lambda e: e.dma_start(out=bandf[0:16, :, :], in_=bass.AP(tensor=Ft, offset=0, ap=[[16, 16], [384, 8], [1, 128]])),
                     reads=["F_d"], writes=["hT"], dma="fl2")
                S.op("dve", lambda e: e.tensor_copy(out=bandx[:], in_=bandf), reads=["hT"], writes=["bandx"])

                for kv in range(2):
                    for hc in range(2):
                        for jp in range(16):
                            mm(pb[1][:, hc:hc + 1], w1[kv][:, jp, hc * 128:(hc + 1) * 128], pos[kv][:, jp:jp + 1],
                               jp == 0, jp == 15, ["w1k" if kv == 0 else "w1v", "posk" if kv == 0 else "posv"], ["pb1"])
                    evac("dve", c1[kv][:], pb[1][:, 0:2], ["pb1"], ["c1_%d" % kv])

                FMQ, FMKC, FMVC, FMKS, FMKW, FMMQ, FMMK, FMMI, FMMF = 0, 512, 768, 1024, 1152, 1280, 1536, 1792, 1796
                TM0, TMV, TMO = 1800, 2080, 2592
                pj = [1]

                def pjbank():
                    pj[0] = 3 - pj[0]
                    return pj[0]

                def proj_fm(lhs_fn, M):
                    bi = pjbank()
                    for k in range(8):
                        mm(pb[bi][0:M, :], lhs_fn(k), hT[:, k, :], k == 0, k == 7, ["w_in", "hT"], ["pb%d" % bi])
                    return bi

                for st in range(NST):
                    for tt in range(4):
                        n = 4 * st + tt
                        sl = 0
                        S.op("sp", lambda e, n=n, sl=sl: e.dma_start(out=xt[sl][:], in_=x_d[n * 128:(n + 1) * 128, :]),
                             writes=["xt%d" % sl], dma="x%d" % sl)
                        rms_prep(xt[sl][:], gpre[:], xs[:], "pre", "xt%d" % sl, reads_extra=["gpre"])
                        transpose8(xs[:], hT[:, :, tt * 128:(tt + 1) * 128], "pre_xs", "hT", bank=pjbank())

                    for g in range(4):
                        bi = proj_fm(lambda k, g=g: w_in[:, k, FMQ + 128 * g:FMQ + 128 * (g + 1)], 128)
                        evac("act", qTz[0][0:64, :, g, :], pb[bi][0:64, :].rearrange("p (a t) -> p a t", a=4), ["pb%d" % bi], ["qT"], scale=0.125)
                        evac("dve", qTz[1][64:128, :, g, :], pb[bi][64:128, :].rearrange("p (a t) -> p a t", a=4), ["pb%d" % bi], ["qT"], scale=0.125)
                    for kv in range(2):
                        for h in range(2):
                            base = (FMKC if kv == 0 else FMVC) + 128 * h
                            key = "kc2_%d%d" % (kv, h)
                            buf = kc2[kv][h]
                            if st > 0:
                                S.op("pool", lambda e, buf=buf: e.tensor_copy(out=buf[:, 0:16], in_=buf[:, 512:528]),
                                     reads=[key], writes=[key])
                            bi = proj_fm(lambda k, base=base: w_in[:, k, base:base + 128], 128)
                            evac("act", buf[0:64, 16:528], pb[bi][0:64, :], ["pb%d" % bi], [key])
                            evac("dve", buf[64:128, 15:527], pb[bi][64:128, :], ["pb%d" % bi], [key])
                    bi = proj_fm(lambda k: w_in[:, k, FMKS:FMKS + 128], 128)
                    evac("act", kTs[0][0:64, st * 512:(st + 1) * 512], pb[bi][0:64, :], ["pb%d" % bi], [("kslc", st)])
                    evac("dve", kTs[1][64:128, st * 512:(st + 1) * 512], pb[bi][64:128, :], ["pb%d" % bi], [("kslc", st)])
                    bi = proj_fm(lambda k: w_in[:, k, FMKW:FMKW + 128], 128)
                    evac(alt(), kT_win[:, st * 512:(st + 1) * 512], pb[bi][:, :], ["pb%d" % bi], [("kwin", st)])
                    if st > 0:
                        S.op("pool", lambda e: e.tensor_copy(out=uT[:, :, 0:3], in_=uT[:, :, 512:515]), reads=["uT"], writes=["uT"])
                    for c in range(4):
                        base = FMMQ + 128 * c
                        bi = proj_fm(lambda k, base=base: w_in[:, k, base:base + 128], 128)
                        evac(alt(), uT[:, c, 3:515], pb[bi][:, :], ["pb%d" % bi], ["uT"])
                    bi = proj_fm(lambda k: w_in[:, k, FMMI:FMMI + 4], 4)
                    S.op("act", lambda e, bi=bi: e.activation(out=gI[:], in_=pb[bi][0:4, :], func=AF.Identity, bias=gb[:, 0:1]),
                         reads=["pb%d" % bi, "gb"], writes=["gI"])
                    bi = proj_fm(lambda k: w_in[:, k, FMMF:FMMF + 4], 4)
                    S.op("act", lambda e, bi=bi: e.activation(out=gE3[:], in_=pb[bi][0:4, :], func=AF.Exp, scale=-1.0, bias=ngb1[:]),
                         reads=["pb%d" % bi, "ngb1"], writes=["gE3"])
                    S.op("act", lambda e: e.activation(out=gE3[:], in_=gE3[:], func=AF.Ln, bias=1.0), reads=["gE3"], writes=["gE3"])
                    S.op("dve", lambda e: e.tensor_tensor_scan(out=gL[:], data0=resetmask[:], data1=gE3[:], initial=0.0,
                                                               op0=ALU.mult, op1=ALU.add),
                         reads=["gE3", "resetmask"], writes=["gL"])
                    S.op("dve", lambda e: e.tensor_add(out=gI[:], in0=gI[:], in1=gL[:]), reads=["gI", "gL"], writes=["gI"])
                    S.op("dve", lambda e: e.tensor_tensor_scan(out=gG[:], data0=negreset[:], data1=gI[:], initial=-1.0e30,
                                                               op0=ALU.add, op1=ALU.max),
                         reads=["gI", "negreset"], writes=["gG"])
                    bLv, GbLv, mnx, mst, Mend, dec, carry = (gsm[:, 0:8], gsm[:, 8:16], gsm[:, 16:24], gsm[:, 24:32],
                                                             gsm[:, 32:40], gsm[:, 40:48], gsm[:, 48:49])
                    S.op("dve", lambda e: e.tensor_scalar_mul(out=bLv, in0=gL[:, 63:512:64], scalar1=-1.0), reads=["gL"], writes=["gsm"])
                    S.op("dve", lambda e: e.tensor_add(out=GbLv, in0=gG[:, 63:512:64], in1=bLv), reads=["gG", "gsm"], writes=["gsm"])
                    S.op("dve", lambda e: e.tensor_tensor_scan(out=mnx, data0=bLv, data1=GbLv, initial=carry, op0=ALU.add, op1=ALU.max),
                         reads=["gsm"], writes=["gsm"])
                    S.op("dve", lambda e: e.tensor_copy(out=gsm[:, 24:25], in_=carry), reads=["gsm"], writes=["gsm"])
                    S.op("dve", lambda e: e.tensor_copy(out=gsm[:, 25:32], in_=gsm[:, 16:23]), reads=["gsm"], writes=["gsm"])
                    S.op("dve", lambda e: e.tensor_copy(out=carry, in_=gsm[:, 23:24]), reads=["gsm"], writes=["gsm"])
                    S.op("dve", lambda e: e.tensor_max(out=Mend, in0=mst, in1=gG[:, 63:512:64]), reads=["gsm", "gG"], writes=["gsm"])
                    S.op("dve", lambda e: e.tensor_sub(out=dec, in0=mst, in1=Mend), reads=["gsm"], writes=["gsm"])
                    S.op("act", lambda e: e.activation(out=dec, in_=dec, func=AF.Exp), reads=["gsm"], writes=["gsm"])
                    mst_bc = mst.unsqueeze(2).to_broadcast([4, 8, 64])
                    Mend_bc = Mend.unsqueeze(2).to_broadcast([4, 8, 64])
                    v3 = lambda t_: t_[:].rearrange("p (c t) -> p c t", c=8)
                    S.op("dve", lambda e: e.tensor_max(out=v3(gG), in0=v3(gG), in1=mst_bc), reads=["gG", "gsm"], writes=["gG"])
                    S.op("dve", lambda e: e.tensor_sub(out=gRA[:, :, 0, :], in0=Mend_bc, in1=v3(gG)), reads=["gG", "gsm"], writes=["gRA"])
                    S.op("dve", lambda e: e.tensor_sub(out=gRA[:, :, 1, :], in0=mst_bc, in1=v3(gG)), reads=["gG", "gsm"], writes=["gRA"])
                    S.op("act", lambda e: e.activation(out=gRAb[:], in_=gRA[:], func=AF.Exp), reads=["gRA"], writes=["gRAb"])
                    S.op("dve", lambda e: e.tensor_sub(out=gE3[:], in0=gL[:], in1=gG[:]), reads=["gL", "gG"], writes=["gE3"])
                    S.op("act", lambda e: e.activation(out=gE3[:], in_=gE3[:], func=AF.Exp), reads=["gE3"], writes=["gE3"])
                    S.op("dve", lambda e: e.tensor_sub(out=v3(gI), in0=v3(gI), in1=Mend_bc), reads=["gI", "gsm"], writes=["gI"])
                    S.op("act", lambda e: e.activation(out=gI[:], in_=gI[:], func=AF.Exp), reads=["gI"], writes=["gI"])
                    S.op("dve", lambda e: e.tensor_scalar_mul(out=gI[:], in0=gI[:], scalar1=0.125), reads=["gI"], writes=["gI"])
                    for tt in range(4):
                        S.op("pe", lambda e, tt=tt: e.transpose(out=pb[0][:, 0:4], in_=gE3[:, tt * 128:(tt + 1) * 128], identity=ident_f[0:4, 0:4]),
                             reads=["gE3", "ident_f"], writes=["pb0"], fence=True)
                        evac("dve", E3_tm[:, tt, :], pb[0][:, 0:4], ["pb0"], ["E3_tm"])
                        S.op("pe", lambda e, tt=tt: e.transpose(out=pb[0][:, 0:4], in_=gI[:, tt * 128:(tt + 1) * 128], identity=ident_f[0:4, 0:4]),
                             reads=["gI", "ident_f"], writes=["pb0"], fence=True)
                        evac("dve", es_tm[:, tt, :], pb[0][:, 0:4], ["pb0"], ["es_tm"])
                    for cp in range(2):
                        mm(pb[0][:, 0:8], selpair[:, cp, :], dec, True, True, ["selpair", "gsm"], ["pb0"], fence=True)
                        evac("dve", decay_bc[:, cp, :], pb[0][:, 0:8], ["pb0"], ["decay_bc"])

                    for c in range(4):
                        S.op("dve", lambda e, c=c: e.tensor_scalar_mul(out=cacc, in0=uT[:, c, 3:515], scalar1=convw[:, c, 3:4]),
                             reads=["uT", "convw"], writes=["hh"])
                        for j in range(3):
                            S.op("dve", lambda e, c=c, j=j: e.scalar_tensor_tensor(out=cacc, in0=uT[:, c, j:j + 512], scalar=convw[:, c, j:j + 1],
                                                                                   in1=cacc, op0=ALU.mult, op1=ALU.add),
                                 reads=["uT", "convw", "hh"], writes=["hh"])
                        S.op("act", lambda e, c=c: e.activation(out=qkml[:, c, :], in_=cacc, func=AF.Silu, bias=convb[:, c:c + 1]),
                             reads=["hh", "convb"], writes=["qkml"])
                    for tt in range(4):
                        bk_ = pjbank()
                        pv0 = pbf(bk_)
                        for c in range(2):
                            S.op("pe", lambda e, tt=tt, c=c, pv0=pv0: e.transpose(out=pv0[:, c * 128:(c + 1) * 128], in_=qkml[:, 2 + c, tt * 128:(tt + 1) * 128],
                                                                                  identity=ident_b[:]),
                                 reads=["qkml", "ident_b"], writes=["pb%d" % bk_])
                        evac(alt(), k_tm[:, tt, :], pv0[:, 0:256], ["pb%d" % bk_], ["k_tm"])

                    for tt in range(4):
                        n = 4 * st + tt
                        tsl = slice(tt * 128, (tt + 1) * 128)
                        bi = pjbank()
                        for k in range(8):
                            mm(pb[bi][:, 0:280], hT[:, k, tsl], w_in[:, k, TM0:TM0 + 280], k == 0, k == 7, ["w_in", "hT"], ["pb%d" % bi])
                        evac("act", V_slc[:, n, :, 0:64], pb[bi][:, 0:128].rearrange("p (h d) -> p h d", h=2), ["pb%d" % bi], [("vslc", n), "V_slc"])
                        evac("dve", V_win[:, n, :, 0:64], pb[bi][:, 128:256].rearrange("p (h d) -> p h d", h=2), ["pb%d" % bi], [("vwin", n), "V_win"])
                        S.op("act", lambda e, bi=bi, tt=tt: e.activation(out=gates[:, tt, :], in_=pb[bi][:, 256:280], func=AF.Sigmoid),
                             reads=["pb%d" % bi], writes=["gates"])
                        bi = pjbank()
                        for k in range(8):
                            mm(pb[bi][:, :], hT[:, k, tsl], w_in[:, k, TMV:TMV + 512], k == 0, k == 7, ["w_in", "hT"], ["pb%d" % bi])
                        evac("dve", v_ml[:, tt, :, 0:128], pb[bi][:, :].rearrange("p (h d) -> p h d", h=4), ["pb%d" % bi], ["v_ml"])
                        bi = pjbank()
                        for k in range(8):
                            mm(pb[bi][:, :], hT[:, k, tsl], w_in[:, k, TMO:TMO + 512], k == 0, k == 7, ["w_in", "hT"], ["pb%d" % bi])
                        S.op("act", lambda e, bi=bi, tt=tt: e.activation(out=og_n[:, tt, :], in_=pb[bi][:, :], func=AF.Sigmoid),
                             reads=["pb%d" % bi], writes=["og_n"])
                        S.op("pool", lambda e, tt=tt: e.tensor_mul(out=og_n[:, tt, :], in0=og_n[:, tt, :], in1=mln[:]),
                             reads=["og_n", "mln"], writes=["og_n"])

                    r0 = 1 if st == 0 else 0
                    nr_ = 32 - r0
                    i0 = 32 * st - 1 + r0
                    for kv in range(2):
                        for h in range(2):
                            key = "kc2_%d%d" % (kv, h)
                            for hc in range(2):
                                for jp in range(16):
                                    c0_ = 2 * jp + 16 * r0
                                    mm(pb[0][:, 0:nr_], w1[kv][:, jp, hc * 128:(hc + 1) * 128],
                                       kc2[kv][h][:, c0_:c0_ + 16 * (nr_ - 1) + 1:16], jp == 0, jp == 15,
                                       ["w1k" if kv == 0 else "w1v", key], ["pb0"])
                                dst = hid_k[:, h, hc, 0:nr_] if kv == 0 else hidT_v[:, h, hc, i0:i0 + nr_]
                                S.op("act", lambda e, dst=dst, kv=kv, hc=hc: e.activation(out=dst, in_=pb[0][:, 0:nr_], func=AF.Silu,
                                                                                       bias=c1[kv][:, hc:hc + 1]),
                                     reads=["pb0", "c1_%d" % kv], writes=["hid_k" if kv == 0 else "hidT_v"])
                    for h in range(2):
                        for hc in range(2):
                            mm(pb[0][:, 0:nr_], w2k[:, hc, :], hid_k[:, h, hc, 0:nr_], hc == 0, hc == 1, ["w2k", "hid_k"], ["pb0"])
                        evac("dve", k_cmpT[64 * h:64 * h + 64, i0:i0 + nr_], pb[0][64 * h:64 * h + 64, 0:nr_], ["pb0"], ["k_cmpT"])
                        for ch in sorted(set([max(i0, 0) // 128, (i0 + nr_ - 1) // 128])):
                            for hc in range(2):
                                mm(pb[0][:, 0:64], hidT_v[:, h, hc, ch * 128:(ch + 1) * 128], w2v[:, hc, :], hc == 0, hc == 1,
                                   ["hidT_v", "w2v"], ["pb0"])
                            evac("act", vcmp[:, h, ch, 0:64], pb[0][:, 0:64], ["pb0"], ["vcmp"])

                    for tt in range(4):
                        n = 4 * st + tt
                        tsl = slice(tt * 128, (tt + 1) * 128)
                        ysl = 0
                        sbk = [0]

                        def sbank():
                            sbk[0] = (sbk[0] + 1) % 3
                            return (3, 4, 6)[sbk[0]]

                        def zero_bank(bi, ncol):
                            mm(pb[bi][:, 0:ncol], zrow[0:1, 0:128], zrow[0:1, 0:ncol], True, False, ["zrow"], ["pb%d" % bi], skip=True)

                        ebk = [0]

                        def attn_chunk(kT_ap, kkeys, q_ap, extras, pv_fn, last, bank=None):
                            bi = sbank() if bank is None else bank
                            mm(pb[bi][:, :], kT_ap, q_ap, True, len(extras) == 0, kkeys + ["qT"], ["pb%d" % bi])
                            for i_, (l_, r_, ks_) in enumerate(extras):
                                mm(pb[bi][:, :], l_, r_, False, i_ == len(extras) - 1, ks_, ["pb%d" % bi])
                            ebk[0] = (ebk[0] + 1) % 3
                            E = Eb[ebk[0]]
                            S.op("act", lambda e, bi=bi, E=E: e.activation(out=E[:].rearrange("p g t -> p (g t)"), in_=pb[bi][:, :], func=AF.Exp),
                                 reads=["pb%d" % bi], writes=["Eb%d" % ebk[0]])
                            for g in range(4):
                                o_ap, v_ap, oks, vks = pv_fn(g)
                                mm(o_ap, E[:, g, :], v_ap, False, last, ["Eb%d" % ebk[0]] + vks, oks, skip=True)

                        for kvh in range(2):
                            hb = 64 * kvh
                            q_ap = qTz[kvh][:, tt, :, :].rearrange("p g t -> p (g t)")
                            idb4 = ident_b[:].unsqueeze(1).to_broadcast([128, 4, 128])
                            val = 8 * n + 6
                            chunks = [0] + ([1] if val >= 128 else [])
                            zero_bank(5, 258)
                            zero_bank(6, 258)

                            def pv_cmp(g, ch=None):
                                bi = 5 + g // 2
                                return (pb[bi][:, (g % 2) * 129:(g % 2) * 129 + 129], None, ["pb%d" % bi], None)

                            for ci, ch in enumerate(chunks):
                                v_ = val - 128 * ch
                                extras = []
                                if v_ - 15 <= 127:
                                    off = 254 - v_
                                    extras.append((shiftw[:, off:off + 128], bandx[:, 4 * kvh:4 * kvh + 4, :].rearrange("p g t -> p (g t)"), ["shiftw", "bandx"]))

                                def pvf(g, ch=ch):
                                    bi = 5 + g // 2
                                    c0_ = (g % 2) * 129
                                    return (pb[bi][:, c0_:c0_ + 129], vcmp[:, kvh, ch, :], ["pb%d" % bi], ["vcmp"])

                                attn_chunk(k_cmpT[:, ch * 128:(ch + 1) * 128], ["k_cmpT"], q_ap, extras, pvf, ci == len(chunks) - 1, bank=3 + ci)
                            for b2 in range(2):
                                S.op("act", lambda e, b2=b2: e.copy(out=cmp_sb[:, 258 * b2:258 * b2 + 258], in_=pb[5 + b2][:, 0:258]),
                                     reads=["pb%d" % (5 + b2)], writes=["cmp_sb"])
                            Ov = [cmp_sb[:, 0:258].rearrange("p (g c) -> p g c", g=2), cmp_sb[:, 258:516].rearrange("p (g c) -> p g c", g=2)]
                            for b2 in range(2):
                                S.op("dve", lambda e, b2=b2: e.tensor_scalar_max(out=asm[:, 2 * b2:2 * b2 + 2], in0=Ov[b2][:, :, 64], scalar1=1e-30),
                                     reads=["cmp_sb"], writes=["asm"])
                            S.op("dve", lambda e: e.reciprocal(out=asm[:, 0:4], in_=asm[:, 0:4]), reads=["asm"], writes=["asm"])
                            gsl = gates[:, tt, 12 * kvh:12 * kvh + 12]
                            S.op("dve", lambda e, gsl=gsl: e.tensor_mul(out=asm[:, 4:8], in0=asm[:, 0:4], in1=gsl[:, 0:12:3]), reads=["asm", "gates"], writes=["asm"])
                            yv = yacc[:, 256 * kvh:256 * kvh + 256].rearrange("p (g d) -> p g d", g=4)
                            for b2 in range(2):
                                S.op("dve", lambda e, b2=b2: e.tensor_mul(out=yv[:, 2 * b2:2 * b2 + 2, :], in0=Ov[b2][:, :, 0:64],
                                                                          in1=asm[:, 4 + 2 * b2:6 + 2 * b2].unsqueeze(2).to_broadcast([128, 2, 64])),
                                     reads=["cmp_sb", "asm"], writes=["yacc"])
                            for g in range(4):
                                src = Ov[g // 2][:, g % 2, 65:129]
                                if g == 0:
                                    S.op("dve", lambda e, src=src: e.tensor_scalar_mul(out=imp[:], in0=src, scalar1=asm[:, 0:1]),
                                         reads=["cmp_sb", "asm"], writes=["imp"])
                                else:
                                    S.op("dve", lambda e, src=src, g=g: e.scalar_tensor_tensor(out=imp[:], in0=src, scalar=asm[:, g:g + 1], in1=imp[:],
                                                                                               op0=ALU.mult, op1=ALU.add),
                                         reads=["cmp_sb", "asm", "imp"], writes=["imp"])
                            def combine(br, bi):
                                O4 = pb[bi][:, 0:260].rearrange("p (g c) -> p g c", g=4)
                                S.op("dve", lambda e, O4=O4: e.tensor_scalar_max(out=asm[:, 8:12], in0=O4[:, :, 64], scalar1=1e-30),
                                     reads=["pb%d" % bi], writes=["asm"])
                                S.op("dve", lambda e: e.reciprocal(out=asm[:, 8:12], in_=asm[:, 8:12]), reads=["asm"], writes=["asm"])
                                S.op("dve", lambda e, gsl=gsl, br=br: e.tensor_mul(out=asm[:, 12:16], in0=asm[:, 8:12], in1=gsl[:, br:12:3]),
                                     reads=["asm", "gates"], writes=["asm"])
                                S.op("dve", lambda e, O4=O4: e.tensor_mul(out=ytmp, in0=O4[:, :, 0:64],
                                                                         in1=asm[:, 12:16].unsqueeze(2).to_broadcast([128, 4, 64])),
                                     reads=["pb%d" % bi, "asm"], writes=["cmp_sb"])
                                S.op("pool", lambda e, yv=yv: e.tensor_add(out=yv, in0=yv, in1=ytmp), reads=["cmp_sb", "yacc"], writes=["yacc"])
                            zero_bank(5, 260)

                            def pv_win(g, kc):
                                return (pb[5][:, g * 65:g * 65 + 65], V_win[:, kc, kvh, :], ["pb5"], [("vwin", kc)])

                            for kc in range(max(0, n - 4), n + 1):
                                extras = []
                                if kc == n:
                                    extras.append((anti_b[:], G0[:, 4 * kvh:4 * kvh + 4, :].rearrange("p g t -> p (g t)"), ["anti_b", "G0"]))
                                elif kc == n - 1:
                                    extras.append((anti_b[:], G1[:, 4 * kvh:4 * kvh + 4, :].rearrange("p g t -> p (g t)"), ["anti_b", "G1"]))
                                elif kc == n - 4:
                                    extras.append((ident_b[:], d4[:].unsqueeze(1).to_broadcast([128, 4, 128]), ["ident_b", "d4"]))
                                attn_chunk(kT_win[:, kc * 128:(kc + 1) * 128], [("kwin", kc // 4)], q_ap, extras,
                                           lambda g, kc=kc: pv_win(g, kc), kc == n)
                            combine(2, 5)
                            w0 = 62 - 2 * n
                            S.op("dve", lambda e, w0=w0: e.tensor_mul(out=sc[:], in0=imp[:], in1=vm_tab[:, w0:w0 + 64]), reads=["imp", "vm_tab"], writes=["sc"])
                            S.op("dve", lambda e, w0=w0: e.tensor_add(out=sc[:], in0=sc[:], in1=add_tab[:, w0:w0 + 64]), reads=["sc", "add_tab"], writes=["sc"])
                            S.op("dve", lambda e: e.memset(sc[:, 0:1], 1.0e4), reads=["sc"], writes=["sc"])
                            S.op("dve", lambda e: e.max(out=m8[:, 0:8], in_=sc[:]), reads=["sc"], writes=["m8"])
                            S.op("dve", lambda e: e.match_replace(out=sc2[:], in_to_replace=m8[:, 0:8], in_values=sc[:], imm_value=-2.0),
                                 reads=["sc", "m8"], writes=["imp"])
                            S.op("dve", lambda e: e.max(out=m8[:, 8:16], in_=sc2[:]), reads=["imp"], writes=["m8"])
                            S.op("dve", lambda e: e.tensor_scalar(out=negsel[:], in0=sc[:], scalar1=m8[:, 15:16], scalar2=NEG, op0=ALU.is_lt, op1=ALU.mult),
                                 reads=["sc", "m8"], writes=["negsel"])
                            oh = 64 * (1 - kvh)
                            nb_ = sbank()
                            S.op("pe", lambda e, oh=oh, nb_=nb_: e.transpose(out=pbf(nb_)[oh:oh + 64, 0:128], in_=negsel[:], identity=ident_b[:]),
                                 reads=["negsel", "ident_b"], writes=["pb%d" % nb_])
                            S.op("dve", lambda e, oh=oh, kvh=kvh, nb_=nb_: e.tensor_copy(out=qTs[kvh][oh:oh + 64, :, :],
                                                                                       in_=pbf(nb_)[oh:oh + 64, 0:128].unsqueeze(1).to_broadcast([64, 4, 128])),
                                 reads=["pb%d" % nb_], writes=["qTs%d" % kvh])
                            S.op("pool", lambda e, hb=hb, kvh=kvh, tsl=tsl: e.tensor_copy(out=qTs[kvh][hb:hb + 64, :, :], in_=qTz[kvh][hb:hb + 64, tt, :, :]),
                                 reads=["qT"], writes=["qTs%d" % kvh])
                            zero_bank(5, 260)

                            def pv_sel(g, kc):
                                return (pb[5][:, g * 65:g * 65 + 65], V_slc[:, kc, kvh, :], ["pb5"], [("vslc", kc)])

                            for kc in range(n + 1):
                                extras = []
                                if kc == n:
                                    extras.append((anti_b[:], G0[:, 4 * kvh:4 * kvh + 4, :].rearrange("p g t -> p (g t)"), ["anti_b", "G0"]))
                                else:
                                    if kc == n - 1:
                                        extras.append((anti_b[:], G1[:, 4 * kvh:4 * kvh + 4, :].rearrange("p g t -> p (g t)"), ["anti_b", "G1"]))
                                attn_chunk(kTs[kvh][:, kc * 128:(kc + 1) * 128], [("kslc", kc // 4), "kTs_e%d" % kvh, "qTs%d" % kvh], qTs[kvh][:].rearrange("p g t -> p (g t)"), extras,
                                           lambda g, kc=kc: pv_sel(g, kc), kc == n)
                            combine(1, 5)
                        S.op("act", lambda e, ysl=ysl: e.copy(out=ycat[ysl][:, 0:512], in_=yacc[:]), reads=["yacc"], writes=["ycat%d" % ysl])

                        for hp in range(2):
                            for cc in range(2):
                                cg = tt * 2 + cc
                                csl = slice(tt * 128 + cc * 64, tt * 128 + cc * 64 + 64)
                                pr = slice(64 * cc, 64 * cc + 64)
                                for hh_ in range(2):
                                    h = 2 * hp + hh_
                                    hbq = 64 * hh_
                                    hr = slice(hbq, hbq + 64)
                                    mm(pb[0][hr, 0:128], selbc[:, h, :], gRAb[:, cg, :, :].rearrange("p a t -> p (a t)"), True, True,
                                       ["selbc", "gRAb"], ["pb0"], fence=True)
                                    S.op("dve", lambda e, hr=hr, hp=hp, csl=csl: e.tensor_mul(
                                        out=qba[hr, :, :], in0=qkml[hr, hp, csl].unsqueeze(1).to_broadcast([64, 2, 64]),
                                        in1=pb[0][hr, 0:128].rearrange("p (a t) -> p a t", a=2)),
                                        reads=["pb0", "qkml"], writes=["qba"])
                                    S.op("act", lambda e, hr=hr: e.copy(out=qbb[hr, :], in_=qba[hr, 0, :]), reads=["qba"], writes=["qbb"])
                                    mm(pb[0][pr, 128:192], qkml[hr, 2 + hp, csl], qbb[hr, :], True, True, ["qkml", "qbb"], ["pb0"], fence=True)
                                    S.op("dve", lambda e, pr=pr, tt=tt, h=h: e.scalar_tensor_tensor(
                                        out=STb[pr, :], in0=pb[0][pr, 128:192], scalar=es_tm[pr, tt, h:h + 1], in1=tri_ml[pr, :],
                                        op0=ALU.mult, op1=ALU.mult),
                                        reads=["pb0", "es_tm", "tri_ml"], writes=["STb"])
                                    mm(pb[7][pr, hh_ * 129:hh_ * 129 + 129], STb[pr, :], v_ml[pr, tt, h, :], True, False, ["STb", "v_ml"], ["pb7"], fence=True)
                                    mm(pb[7][pr, hh_ * 129:hh_ * 129 + 129], qba[hr, 1, :], Cst[hr, hp, :], False, True, ["qba", "Cst"], ["pb7"], fence=True)
                                S.op("dve", lambda e, pr=pr, tt=tt, hp=hp: e.tensor_mul(
                                    out=kw[pr, 0:128].rearrange("p (h d) -> p h d", h=2),
                                    in0=k_tm[pr, tt, 128 * hp:128 * hp + 128].rearrange("p (h d) -> p h d", h=2),
                                    in1=es_tm[pr, tt, 2 * hp:2 * hp + 2].unsqueeze(2).to_broadcast([64, 2, 64])),
                                    reads=["k_tm", "es_tm"], writes=["kw"])
                                for hh_ in range(2):
                                    h = 2 * hp + hh_
                                    mm(pb[7][64 * hh_:64 * hh_ + 64, 258:387], kw[pr, 64 * hh_:64 * hh_ + 64], v_ml[pr, tt, h, :], True, True,
                                       ["kw", "v_ml"], ["pb7"], fence=True)
                                S.op("dve", lambda e, hp=hp, cg=cg: e.scalar_tensor_tensor(
                                    out=Cst[:, hp, :], in0=Cst[:, hp, :], scalar=decay_bc[:, hp, cg:cg + 1], in1=pb[7][:, 258:387],
                                    op0=ALU.mult, op1=ALU.add),
                                    reads=["Cst", "decay_bc", "pb7"], writes=["Cst"])
                            Hv = pb[7][:, 0:258].rearrange("p (h c) -> p h c", h=2)
                            S.op("act", lambda e, Hv=Hv: e.activation(out=mls[:, 0:2], in_=Hv[:, :, 128], func=AF.Abs),
                                 reads=["pb7"], writes=["mls"])
                            S.op("dve", lambda e, tt=tt, hp=hp: e.tensor_max(out=mls[:, 0:2], in0=mls[:, 0:2], in1=E3_tm[:, tt, 2 * hp:2 * hp + 2]),
                                 reads=["mls", "E3_tm"], writes=["mls"])
                            S.op("dve", lambda e: e.reciprocal(out=mls[:, 0:2], in_=mls[:, 0:2]), reads=["mls"], writes=["mls"])
                            S.op("dve", lambda e, Hv=Hv, hp=hp: e.tensor_mul(out=hh[:, 2 * hp:2 * hp + 2, :], in0=Hv[:, :, 0:128],
                                                                             in1=mls[:, 0:2].unsqueeze(2).to_broadcast([128, 2, 128])),
                                 reads=["pb7", "mls"], writes=["hh"])
                        for h in range(4):
                            S.op("act", lambda e, h=h: e.activation(out=junk[:, 0:128], in_=hh[:, h, :], func=AF.Square, accum_out=mls[:, 4 + h:5 + h]),
                                 reads=["hh"], writes=["junk", "mls"])
                        S.op("act", lambda e: e.activation(out=mls[:, 4:8], in_=mls[:, 4:8], func=AF.Ln, scale=1.0 / 128, bias=1e-6),
                             reads=["mls"], writes=["mls"])
                        S.op("act", lambda e: e.activation(out=mls[:, 4:8], in_=mls[:, 4:8], func=AF.Exp, scale=-0.5), reads=["mls"], writes=["mls"])
                        S.op("dve", lambda e: e.tensor_mul(out=hh[:], in0=hh[:], in1=mls[:, 4:8].unsqueeze(2).to_broadcast([128, 4, 128])),
                             reads=["hh", "mls"], writes=["hh"])
                        S.op("pool", lambda e, tt=tt, ysl=ysl: e.tensor_mul(out=ycat[ysl][:, 512:1024], in0=hh[:].rearrange("p h d -> p (h d)"), in1=og_n[:, tt, :]),
                             reads=["hh", "og_n"], writes=["ycat%d" % ysl])
                        S.op("sp", lambda e, n=n, ysl=ysl: e.dma_start(out=ycat_d[n * 128:(n + 1) * 128, :], in_=ycat[ysl][:]),
                             reads=["ycat%d" % ysl], writes=[("ycat_d", n)], dma="yo%d" % ysl)
                        if debug:
                            S.op("sp", lambda e, n=n, ysl=ysl: e.dma_start(out=dbg["ycat"][n * 128:(n + 1) * 128, :], in_=ycat[ysl][:]),
                                 reads=["ycat%d" % ysl], dma="dyo%d" % ysl)

        def load_w(dst, src_d, nchunk, key, sem):
            for c in range(nchunk):
                S.op("pool", lambda e, c=c: e.dma_start(out=dst[:, c, :], in_=src_d[c * 128:(c + 1) * 128, :]),
                     writes=[(key, c)], dma=sem)
            return [(key, c) for c in range(nchunk)]

        def load_bc(dst, src_d, key, sem):
            S.op("sp", lambda e: e.dma_start(out=dst[:], in_=src_d.partition_broadcast(128)), writes=[key], dma=sem)

        def postnorm(banks, g_bc, tmp, tag, tmpkey):
            s2 = small[tag]
            for hf in range(2):
                S.op("act", lambda e, hf=hf: e.activation(out=tmp[:, hf * 512:(hf + 1) * 512], in_=pb[banks[hf]][:, :], func=AF.Square,
                                                          accum_out=s2[:, hf:hf + 1]),
                     reads=["pb%d" % banks[hf]], writes=[tmpkey, tag])
            S.op("dve", lambda e: e.tensor_add(out=s2[:, 2:3], in0=s2[:, 0:1], in1=s2[:, 1:2]), reads=[tag], writes=[tag])
            S.op("act", lambda e: e.activation(out=s2[:, 2:3], in_=s2[:, 2:3], func=AF.Ln, scale=1.0 / D, bias=1e-6), reads=[tag], writes=[tag])
            S.op("act", lambda e: e.activation(out=s2[:, 2:3], in_=s2[:, 2:3], func=AF.Exp, scale=-0.5), reads=[tag], writes=[tag])
            for hf in range(2):
                S.op("dve", lambda e, hf=hf: e.scalar_tensor_tensor(out=tmp[:, hf * 512:(hf + 1) * 512], in0=pb[banks[hf]][:, :], scalar=s2[:, 2:3],
                                                                    in1=g_bc[:, hf * 512:(hf + 1) * 512], op0=ALU.mult, op1=ALU.mult),
                     reads=["pb%d" % banks[hf], tag, tag + "_g"], writes=[tmpkey])

        if "B1" in phases:
            S.barrier()
            with contextlib.ExitStack() as pbx:
                Bq = lambda name, shape, dt: sbuf(pbx, name, shape, dt)
                w_out = Bq("w_out", [128, 8, D], BF16)
                w_xq = Bq("w_xq", [128, 8, D], BF16)
                w_xo = Bq("w_xo", [128, 8, D], BF16)
                gpost = Bq("gpost", [128, D], F32)
                gxpre = Bq("gxpre", [128, D], F32)
                gxpost = Bq("gxpost", [128, D], F32)
                ones_b = Bq("ones_b", [128, 128], BF16)
                kxT = Bq("kxT", [128, 8, 256], BF16)
                vx = Bq("vx", [128, 2, D], BF16)
                xin = [Bq("xin0", [128, D], F32), Bq("xin1", [128, D], F32)]
                xsbs = [Bq("xsb0", [128, D], BF16), Bq("xsb1", [128, D], BF16)]
                xsb = xsbs[0]
                for nm in ("pn1", "pn2", "xpre_ss", "mem_ss"):
                    small[nm] = Bq(nm, [128, 4], F32) if nm.startswith("pn") else Bq(nm, [128, 1], F32)
                inner = contextlib.ExitStack()
                Bi = lambda name, shape, dt: sbuf(inner, name, shape, dt)
                w_xkv = Bi("w_xkv", [128, 8, 2 * D], BF16)
                gmem = Bi("gmem", [128, D], F32)
                memT = Bi("memT", [128, 8, 256], BF16)
                kw_out = load_w(w_out, w_out_d, 8, "w_out", "b_wout")
                kw_xkv = load_w(w_xkv, w_xkv_d, 8, "w_xkv", "b_wxkv")
                kw_xq = load_w(w_xq, w_xq_d, 8, "w_xq", "b_wxq")
                kw_xo = load_w(w_xo, w_xo_d, 8, "w_xo", "b_wxo")
                load_bc(gpost, gpost_d, "pn1_g", "b_g1")
                load_bc(gxpre, gxpre_d, "gxpre", "b_g2")
                load_bc(gxpost, gxpost_d, "pn2_g", "b_g3")
                load_bc(gmem, gmem_d, "gmem", "b_g4")
                S.op("sp", lambda e: e.dma_start(out=ones_b[:], in_=cd["ones_b"]), writes=["ones_b"], dma="b_ones")
                for mt in range(2):
                    S.op("sp", lambda e, mt=mt: e.dma_start(out=xin[mt][:], in_=mem_d[mt * 128:(mt + 1) * 128, :]), writes=["xin%d" % mt], dma="b_x%d" % mt)
                    rms_prep(xin[mt][:], gmem[:], xsb[:], "mem", "xin%d" % mt, reads_extra=["gmem"], xskey="xsb0")
                    transpose8(xsb[:], memT[:, :, mt * 128:(mt + 1) * 128], "xsb0", "memT")
                for fc in range(8):
                    bi = 1 + fc % 2
                    for k in range(8):
                        mm(pb[bi][:, 0:256], w_xkv[:, k, fc * 128:(fc + 1) * 128], memT[:, k, :], k == 0, k == 7, kw_xkv + ["memT"], ["pb%d" % bi])
                    evac(alt(), kxT[:, fc, :], pb[bi][:, 0:256], ["pb%d" % bi], ["kxT"])
                for mc in range(2):
                    for hf in range(2):
                        bi = 1 + hf
                        for k in range(8):
                            mm(pb[bi][:, :], memT[:, k, mc * 128:(mc + 1) * 128], w_xkv[:, k, D + hf * 512:D + (hf + 1) * 512], k == 0, k == 7,
                               kw_xkv + ["memT"], ["pb%d" % bi])
                        evac(alt(), vx[:, mc, hf * 512:(hf + 1) * 512], pb[bi][:, :], ["pb%d" % bi], ["vx"])

                inner.close()
                S.barrier()
                yb = [Bq("yb0", [128, D], BF16), Bq("yb1", [128, D], BF16)]
                yTs = [Bq("yT0", [128, 8, 128], BF16), Bq("yT1", [128, 8, 128], BF16)]
                x1b = [Bq("x1a", [128, 4, D], F32), Bq("x1b", [128, 4, D], F32)]
                h2T = Bq("h2T", [128, 8, 512], BF16)
                qxT = Bq("qxT", [128, 8, 512], BF16)
                Ex = [Bq("Ex0", [128, 2, 512], BF16), Bq("Ex1", [128, 2, 512], BF16)]
                rZbs = [Bq("rZb0", [128, 512], F32), Bq("rZb1", [128, 512], F32)]
                oxT = Bq("oxT", [128, 8, 512], BF16)
                tmpbs = [Bq("tmpb0", [128, D], F32), Bq("tmpb1", [128, D], F32), Bq("tmpb2", [128, D], F32)]
                x2o = [Bq("x2o0", [128, D], F32), Bq("x2o1", [128, D], F32)]
                bpair = [0]

                def next_pair():
                    bpair[0] ^= 1
                    return (1, 2) if bpair[0] else (3, 4)

                for gi in range(NST):
                    x1 = x1b[gi % 2]
                    x1k = "x1%d" % (gi % 2)
                    for tt in range(4):
                        n = 4 * gi + tt
                        sl = n % 2
                        tsl = slice(tt * 128, (tt + 1) * 128)
                        S.op("sp", lambda e, n=n, sl=sl: e.dma_start(out=yb[sl][:], in_=ycat_d[n * 128:(n + 1) * 128, :]), writes=["yb%d" % sl], dma="b_y%d" % sl)
                        S.op("sp", lambda e, n=n, sl=sl: e.dma_start(out=xin[sl][:], in_=x_d[n * 128:(n + 1) * 128, :]), writes=["xin%d" % sl], dma="b_x%d" % sl)
                        yT = yTs[sl]
                        ytk = "yT%d" % sl
                        tmpb = tmpbs[sl]
                        tk = "tmpb%d" % sl
                        xsb_ = xsbs[sl]
                        xk = "xsb%d" % sl
                        transpose8(yb[sl][:], yT[:], "yb%d" % sl, ytk)
                        bp = next_pair()
                        for hf in range(2):
                            for c in range(8):
                                mm(pb[bp[hf]][:, :], yT[:, c, :], w_out[:, c, hf * 512:(hf + 1) * 512], c == 0, c == 7, kw_out + [ytk], ["pb%d" % bp[hf]])
                        postnorm(bp, gpost, tmpb, "pn1", tk)
                        S.op("pool", lambda e, tt=tt, sl=sl, x1=x1, tmpb=tmpb: e.tensor_add(out=x1[:, tt, :], in0=xin[sl][:], in1=tmpb[:]),
                             reads=["xin%d" % sl, tk], writes=[(x1k, tt)])
                        rms_prep(x1[:, tt, :], gxpre[:], xsb_[:], "xpre", (x1k, tt), reads_extra=["gxpre"], xskey=xk)
                        transpose8(xsb_[:], h2T[:, :, tsl], xk, "h2T", bank=7)
                    for fc in range(8):
                        bi = 1 + fc % 4
                        for k in range(8):
                            mm(pb[bi][:, :], w_xq[:, k, fc * 128:(fc + 1) * 128], h2T[:, k, :], k == 0, k == 7, kw_xq + ["h2T"], ["pb%d" % bi])
                        evac(alt(), qxT[:, fc, :], pb[bi][:, :], ["pb%d" % bi], [("qxT", fc // 2)])
                    for hx in range(4):
                        E = Ex[hx % 2]
                        ek = "Ex%d" % (hx % 2)
                        rZb = rZbs[hx % 2]
                        rk = "rZb%d" % (hx % 2)
                        sb2, zb = ((5, 6), 7) if hx % 2 == 0 else ((1, 2), 3)
                        for mc in range(2):
                            bi = sb2[mc]
                            for dc in range(2):
                                mm(pb[bi][:, :], kxT[:, 2 * hx + dc, mc * 128:(mc + 1) * 128], qxT[:, 2 * hx + dc, :], dc == 0, dc == 1,
                                   ["kxT", ("qxT", hx)], ["pb%d" % bi])
                            S.op("act", lambda e, bi=bi, E=E, mc=mc: e.activation(out=E[:, mc, :], in_=pb[bi][:, :], func=AF.Exp, scale=1.0 / 16),
                                 reads=["pb%d" % bi], writes=[ek])
                        for mc in range(2):
                            mm(pb[zb][:, :], ones_b[:], E[:, mc, :], mc == 0, mc == 1, ["ones_b", ek], ["pb%d" % zb])
                        S.op("act", lambda e, zb=zb, rZb=rZb: e.activation(out=rZb[:], in_=pb[zb][:, :], func=AF.Ln), reads=["pb%d" % zb], writes=[rk])
                        S.op("act", lambda e, rZb=rZb: e.activation(out=rZb[:], in_=rZb[:], func=AF.Exp, scale=-1.0), reads=[rk], writes=[rk])
                        for dvc in range(2):
                            bi = sb2[dvc]
                            for mc in range(2):
                                c0_ = hx * 256 + dvc * 128
                                mm(pb[bi][:, :], vx[:, mc, c0_:c0_ + 128], E[:, mc, :], mc == 0, mc == 1, ["vx", ek], ["pb%d" % bi])
                            S.op("dve", lambda e, bi=bi, hx=hx, dvc=dvc, rZb=rZb: e.tensor_mul(out=oxT[:, 2 * hx + dvc, :], in0=pb[bi][:, :], in1=rZb[:]),
                                 reads=["pb%d" % bi, rk], writes=["oxT"])
                    for tt in range(4):
                        n = 4 * gi + tt
                        sl = n % 2
                        tsl = slice(tt * 128, (tt + 1) * 128)
                        bp = next_pair()
                        for hf in range(2):
                            for c in range(8):
                                mm(pb[bp[hf]][:, :], oxT[:, c, tsl], w_xo[:, c, hf * 512:(hf + 1) * 512], c == 0, c == 7, kw_xo + ["oxT"], ["pb%d" % bp[hf]])
                        postnorm(bp, gxpost, tmpbs[2], "pn2", "tmpb2")
                        S.op("dve", lambda e, tt=tt, sl=sl, x1=x1: e.tensor_add(out=x2o[sl][:], in0=x1[:, tt, :], in1=tmpbs[2][:]),
                             reads=[(x1k, tt), "tmpb2"], writes=["x2o%d" % sl])
                        S.op("sp", lambda e, n=n, sl=sl: e.dma_start(out=x2_d[n * 128:(n + 1) * 128, :], in_=x2o[sl][:]),
                             reads=["x2o%d" % sl], writes=[("x2_d", n)], dma="b_o%d" % sl)
                        if debug:
                            S.op("sp", lambda e, n=n, sl=sl: e.dma_start(out=dbg["x2"][n * 128:(n + 1) * 128, :], in_=x2o[sl][:]),
                                 reads=["x2o%d" % sl], dma="b_do%d" % sl)

        if "B2" in phases:
            S.barrier()
            with contextlib.ExitStack() as pcx:
                Cq = lambda name, shape, dt: sbuf(pcx, name, shape, dt)
                w_gu = Cq("w_gu", [128, 8, 5632], BF16)
                w_dn = Cq("w_dn", [128, 22, D], BF16)
                gfpre = Cq("gfpre", [128, D], F32)
                gfpost = Cq("gfpost", [128, D], F32)
                xin = [Cq("cxin0", [128, D], F32), Cq("cxin1", [128, D], F32)]
                xres = [Cq("xres0", [128, D], F32), Cq("xres1", [128, D], F32)]
                xsb = Cq("cxsb", [128, D], BF16)
                h3T = Cq("h3T", [128, 8, 512], BF16)
                actT = Cq("actT", [128, 22, 512], BF16)
                sgb = [Cq("sgb0", [128, 512], F32), Cq("sgb1", [128, 512], F32)]
                tmpc = Cq("tmpc", [128, D], F32)
                small["pn3"] = Cq("pn3", [128, 4], F32)
                small["fpre_ss"] = Cq("fpre_ss", [128, 1], F32)
                kw_gu = load_w(w_gu, w_gu_d, 8, "w_gu", "c_wgu")
                kgu = {i_: kw_gu for i_ in range(11)}
                kw_dn = load_w(w_dn, w_dn_d, 22, "w_dn", "c_wdn")
                load_bc(gfpre, gfpre_d, "gfpre", "c_g1")
                load_bc(gfpost, gfpost_d, "pn3_g", "c_g2")
                for gi in range(NST):
                    for tt in range(4):
                        n = 4 * gi + tt
                        sl = n % 2
                        S.op("sp", lambda e, n=n, sl=sl: e.dma_start(out=xin[sl][:], in_=x2_d[n * 128:(n + 1) * 128, :]),
                             reads=[("x2_d", n)], writes=["cxin%d" % sl], dma="c_x%d" % sl)
                        rms_prep(xin[sl][:], gfpre[:], xsb[:], "fpre", "cxin%d" % sl, reads_extra=["gfpre"])
                        transpose8(xsb[:], h3T[:, :, tt * 128:(tt + 1) * 128], "fpre_xs", "h3T")
                    for fc in range(22):
                        bg, bu = (1, 2) if fc % 2 == 0 else (3, 4)
                        for k in range(8):
                            mm(pb[bg][:, :], w_gu[:, k, fc * 128:(fc + 1) * 128], h3T[:, k, :], k == 0, k == 7, kgu[fc // 2] + ["h3T"], ["pb%d" % bg])
                        for k in range(8):
                            mm(pb[bu][:, :], w_gu[:, k, 2816 + fc * 128:2816 + (fc + 1) * 128], h3T[:, k, :], k == 0, k == 7, kgu[fc // 2] + ["h3T"], ["pb%d" % bu])
                        sg = sgb[fc % 2]
                        S.op("act", lambda e, sg=sg, bg=bg: e.activation(out=sg[:], in_=pb[bg][:, :], func=AF.Silu), reads=["pb%d" % bg], writes=["sgb%d" % (fc % 2)])
                        S.op("dve", lambda e, sg=sg, bu=bu, fc=fc: e.tensor_mul(out=actT[:, fc, :], in0=pb[bu][:, :], in1=sg[:]),
                             reads=["pb%d" % bu, "sgb%d" % (fc % 2)], writes=[("actT", fc)])
                    for tt in range(4):
                        n = 4 * gi + tt
                        sl = n % 2
                        tsl = slice(tt * 128, (tt + 1) * 128)
                        S.op("sp", lambda e, n=n, sl=sl: e.dma_start(out=xres[sl][:], in_=x2_d[n * 128:(n + 1) * 128, :]),
                             reads=[("x2_d", n)], writes=["xres%d" % sl], dma="c_r%d" % sl)
                        bp = (5, 6)
                        for hf in range(2):
                            for fc in range(22):
                                mm(pb[bp[hf]][:, :], actT[:, fc, tsl], w_dn[:, fc, hf * 512:(hf + 1) * 512], fc == 0, fc == 21,
                                   kw_dn + [("actT", fc)], ["pb%d" % bp[hf]])
                        postnorm(bp, gfpost, tmpc, "pn3", "tmpc")
                        S.op("pool", lambda e, sl=sl: e.tensor_add(out=xres[sl][:], in0=xres[sl][:], in1=tmpc[:]),
                             reads=["xres%d" % sl, "tmpc"], writes=["xres%d" % sl])
                        S.op("sp", lambda e, n=n, sl=sl: e.dma_start(out=out_d[n * 128:(n + 1) * 128, :], in_=xres[sl][:]),
                             reads=["xres%d" % sl], dma="c_o%d" % sl)

        S.emit(reorder=REORDER)
    return nc


def w_in_perm():
    cols = []
    for g in range(4):
        cols += list(range(64 * g, 64 * g + 64)) + list(range(64 * (4 + g), 64 * (4 + g) + 64))
    for b0 in (512, 576, 640, 704):
        cols += list(range(b0, b0 + 64)) * 2
    cols += list(range(768, 896)) + list(range(1024, 1152))
    cols += list(range(1304, 1560)) + list(range(1560, 1816))
    cols += list(range(2328, 2332)) + list(range(2332, 2336))
    cols += list(range(896, 1024)) + list(range(1152, 1280)) + list(range(1280, 1304))
    cols += list(range(1816, 2328))
    cols += list(range(2336, 2848))
    assert len(cols) == 3104 and len(set(cols)) == 2848
    return np.array(cols)


def shared_inputs(inp):
    f = lambda a: np.ascontiguousarray(np.asarray(a, dtype=np.float32))
    m = {}
    rb = np.zeros((33, 8), np.float32)
    rb[0:32] = f(inp["rel_bias"]).T
    rb[32] = NEG
    m["rel_biasT"] = rb
    m["w_in_p"] = f(f(inp["w_in"])[0][:, w_in_perm()])
    m["mix_norm_pre"] = f(inp["mix_norm_pre"])
    for nm in ("cmp_pos_k", "cmp_pos_v"):
        p = f(inp[nm])[0]
        m[nm] = f(p.reshape(16, 2, 64).transpose(1, 2, 0).reshape(128, 16))
    for nm in ("cmp_w1_k", "cmp_w1_v", "cmp_w2_k", "cmp_w2_v", "w_out", "w_xq", "w_xkv", "w_xo", "w_gate_up", "w_down"):
        m[nm] = f(inp[nm])[0]
    cw = f(inp["conv_w"])[0]
    m["conv_w_p"] = f(cw.reshape(4, 4, 128).transpose(2, 1, 0))
    m["conv_b_p"] = f(f(inp["conv_b"])[0].reshape(4, 128).T)
    m["gate_bias_p"] = f(f(inp["mlstm_gate_bias"])[0].T)
    for nm in ("mlstm_norm", "mix_norm_post", "xattn_norm_pre", "mem_norm", "xattn_norm_post", "ffn_norm_pre", "ffn_norm_post"):
        m[nm] = f(inp[nm])
    c = host_consts()
    for n_ in CONST_NAMES:
        m["c_" + n_] = c[n_]
    return m


_NC_CACHE = {}


def kernel(**inputs):
    x = np.asarray(inputs["x"], dtype=np.float32)
    mem = np.asarray(inputs["mem"], dtype=np.float32)
    B, S_TOK, _ = x.shape
    if S_TOK not in _NC_CACHE:
        _NC_CACHE[S_TOK] = build_program(S_TOK)
    nc = _NC_CACHE[S_TOK]
    shared = shared_inputs(inputs)
    in_maps = []
    for b in range(B):
        m = dict(shared)
        m["x"] = np.ascontiguousarray(x[b])
        m["mem"] = np.ascontiguousarray(mem[b])
        in_maps.append(m)
    res = run_bass_kernel_spmd(nc, in_maps, core_ids=list(range(B)))
    return np.stack([np.asarray(r["out"], dtype=np.float32) for r in res.results], axis=0)
```
